# Optimizing a Trainium2 kernel written in Bass

```python
import math
import jax, jax.numpy as jnp
from jax import lax
import numpy as np

D_MODEL = 4096
BATCH = 2
SEQ = 4096
DEPTH = 2

HEAD_DIM = 128
MOBA_HEADS = D_MODEL // (4 * HEAD_DIM)
SB_HEADS = D_MODEL // (4 * HEAD_DIM)
SWA_HEADS = D_MODEL // (2 * HEAD_DIM)
SWA_KV_HEADS = max(1, SWA_HEADS // 8)
MOBA_WIDTH = MOBA_HEADS * HEAD_DIM
SB_WIDTH = SB_HEADS * HEAD_DIM
SWA_Q_WIDTH = SWA_HEADS * HEAD_DIM
SWA_KV_WIDTH = SWA_KV_HEADS * HEAD_DIM
MIX_WIDTH = MOBA_WIDTH + SB_WIDTH + SWA_Q_WIDTH
IN_WIDTH = 3 * MOBA_WIDTH + 3 * SB_WIDTH + SWA_Q_WIDTH + 2 * SWA_KV_WIDTH
MOBA_BLOCK = 256
MOBA_TOPK = 3
MOBA_Q_CHUNK = 32
SB_Q_BLOCK = 128
SWA_WINDOW = 128
REL_BUCKETS = 32
REL_MAX_EXACT = 16
REL_MAX_DISTANCE = 128
N_BIAS_HEADS = MOBA_HEADS + SWA_HEADS
D_FF = 7 * D_MODEL // 2
N_EXPERTS = 8
TOP_K = 2
D_EXPERT = D_MODEL
N_DENSE = (DEPTH + 1) // 2
N_MOE = DEPTH // 2
NORM_EPS = 1e-6
ADA_CHUNKS = 6

kernel_name = 'hybrid_moba_stickbreak_swa_block'


def rms_norm(x, g):
    xf = x.astype(jnp.float32)
    y = xf * lax.rsqrt(jnp.mean(xf * xf, axis=-1, keepdims=True) + NORM_EPS)
    return (y * g.astype(jnp.float32)).astype(x.dtype)


def head_rms_norm(o, g):
    nh, dh = o.shape[1], o.shape[3]
    y = o * lax.rsqrt(jnp.mean(o * o, axis=-1, keepdims=True) + NORM_EPS)
    return y * g.astype(jnp.float32).reshape(1, nh, 1, dh)


def split_heads(t, nh):
    b, s, _ = t.shape
    return t.reshape(b, s, nh, HEAD_DIM).transpose(0, 2, 1, 3)


def merge_heads(o):
    b, nh, s, dh = o.shape
    return o.transpose(0, 2, 1, 3).reshape(b, s, nh * dh)


def rel_bucket(dist):
    n = jnp.maximum(dist, 0)
    nf = jnp.maximum(n, 1).astype(jnp.float32)
    large = REL_MAX_EXACT + (jnp.log(nf / REL_MAX_EXACT) / math.log(REL_MAX_DISTANCE / REL_MAX_EXACT) * (REL_BUCKETS - REL_MAX_EXACT)).astype(jnp.int32)
    return jnp.where(n < REL_MAX_EXACT, n, jnp.minimum(large, REL_BUCKETS - 1))


def moba_attention(q, k, v, bias_table):
    b, nh, s, dh = q.shape
    nb = -(-s // MOBA_BLOCK)
    sp = nb * MOBA_BLOCK
    pad = ((0, 0), (0, 0), (0, sp - s), (0, 0))
    qf = jnp.pad(q.astype(jnp.float32), pad)
    kf = jnp.pad(k.astype(jnp.float32), pad)
    vf = jnp.pad(v.astype(jnp.float32), pad)
    k_blocks = kf.reshape(b, nh, nb, MOBA_BLOCK, dh)
    v_blocks = vf.reshape(b, nh, nb, MOBA_BLOCK, dh)
    k_mean = k_blocks.mean(axis=3)
    gate = jnp.einsum('bhtd,bhnd->bhtn', qf, k_mean)
    q_block = jnp.arange(sp) // MOBA_BLOCK
    fully_past = jnp.arange(nb)[None, :] < q_block[:, None]
    gate = jnp.where(fully_past, gate, -jnp.inf)
    n_sel = min(MOBA_TOPK, nb)
    _, sel = lax.top_k(gate, n_sel)
    sel_ok = sel < q_block[:, None]
    qs = qf * (dh ** -0.5)
    bi = jnp.arange(b)[:, None, None, None]
    hi = jnp.arange(nh)[None, :, None, None]
    hb = jnp.arange(nh)[None, :, None, None, None]
    offs = jnp.arange(MOBA_BLOCK)

    def chunk(start):
        t_pos = start + jnp.arange(MOBA_Q_CHUNK)
        qc = lax.dynamic_slice_in_dim(qs, start, MOBA_Q_CHUNK, axis=2)
        selc = lax.dynamic_slice_in_dim(sel, start, MOBA_Q_CHUNK, axis=2)
        okc = lax.dynamic_slice_in_dim(sel_ok, start, MOBA_Q_CHUNK, axis=2)
        k_sel = k_blocks[bi, hi, selc]
        v_sel = v_blocks[bi, hi, selc]
        kpos = selc[..., None] * MOBA_BLOCK + offs
        bias_sel = bias_table[rel_bucket(t_pos[:, None, None] - kpos), hb].astype(jnp.float32)
        s_sel = jnp.einsum('bhcd,bhcnkd->bhcnk', qc, k_sel) + bias_sel
        s_sel = jnp.where(okc[..., None], s_sel, -jnp.inf).reshape(b, nh, MOBA_Q_CHUNK, n_sel * MOBA_BLOCK)
        own0 = (start // MOBA_BLOCK) * MOBA_BLOCK
        k_own = lax.dynamic_slice_in_dim(kf, own0, MOBA_BLOCK, axis=2)
        v_own = lax.dynamic_slice_in_dim(vf, own0, MOBA_BLOCK, axis=2)
        kpos_own = own0 + offs
        bias_own = bias_table[rel_bucket(t_pos[:, None] - kpos_own[None, :])].astype(jnp.float32).transpose(2, 0, 1)
        s_own = jnp.einsum('bhcd,bhkd->bhck', qc, k_own) + bias_own
        s_own = jnp.where(kpos_own[None, :] <= t_pos[:, None], s_own, -jnp.inf)
        p = jax.nn.softmax(jnp.concatenate([s_sel, s_own], axis=-1), axis=-1)
        p_sel = p[..., :n_sel * MOBA_BLOCK].reshape(b, nh, MOBA_Q_CHUNK, n_sel, MOBA_BLOCK)
        p_own = p[..., n_sel * MOBA_BLOCK:]
        return jnp.einsum('bhcnk,bhcnkd->bhcd', p_sel, v_sel) + jnp.einsum('bhck,bhkd->bhcd', p_own, v_own)

    out = lax.map(chunk, jnp.arange(0, sp, MOBA_Q_CHUNK))
    out = out.transpose(1, 2, 0, 3, 4).reshape(b, nh, sp, dh)
    return out[:, :, :s]


def stick_breaking_attention(q, k, v):
    b, nh, s, dh = q.shape
    qf = q.astype(jnp.float32) * (dh ** -0.5)
    kf = k.astype(jnp.float32)
    vf = v.astype(jnp.float32)
    kpos = jnp.arange(s)

    def block(start):
        qc = lax.dynamic_slice_in_dim(qf, start, SB_Q_BLOCK, axis=2)
        z = jnp.einsum('bhqd,bhkd->bhqk', qc, kf)
        t_pos = start + jnp.arange(SB_Q_BLOCK)
        strict = kpos[None, :] < t_pos[:, None]
        log_keep = jnp.where(strict, jax.nn.log_sigmoid(-z), 0.0)
        after = lax.cumsum(log_keep, axis=3, reverse=True) - log_keep
        a = jnp.where(strict, jnp.exp(jax.nn.log_sigmoid(z) + after), 0.0)
        return jnp.einsum('bhqk,bhkd->bhqd', a, vf)

    out = lax.map(block, jnp.arange(0, s, SB_Q_BLOCK))
    return out.transpose(1, 2, 0, 3, 4).reshape(b, nh, s, dh)


def swa_sink_attention(q, k, v, bias_table, sinks):
    b, hq, s, dh = q.shape
    hkv = k.shape[1]
    g = hq // hkv
    w = SWA_WINDOW
    nb = s // w
    qf = q.astype(jnp.float32).reshape(b, hkv, g, nb, w, dh) * (dh ** -0.5)
    kf = k.astype(jnp.float32).reshape(b, hkv, nb, w, dh)
    vf = v.astype(jnp.float32).reshape(b, hkv, nb, w, dh)
    prev = lambda a: jnp.pad(a, ((0, 0), (0, 0), (1, 0), (0, 0), (0, 0)))[:, :, :-1]
    k_band = jnp.concatenate([prev(kf), kf], axis=3)
    v_band = jnp.concatenate([prev(vf), vf], axis=3)
    scores = jnp.einsum('bkgnqd,bknsd->bkgnqs', qf, k_band)
    i = jnp.arange(w)[:, None]
    j = jnp.arange(2 * w)[None, :]
    dist = i + w - j
    in_window = (dist >= 0) & (dist < w)
    blk_ok = (jnp.arange(nb)[:, None, None] > 0) | (j >= w)[None]
    mask = in_window[None] & blk_ok
    bias = bias_table[rel_bucket(dist)].astype(jnp.float32).transpose(2, 0, 1).reshape(hkv, g, 1, w, 2 * w)
    logits = jnp.where(mask, scores + bias, -jnp.inf)
    sink = sinks.astype(jnp.float32).reshape(1, hkv, g, 1, 1, 1)
    m = jnp.maximum(jnp.max(logits, axis=-1, keepdims=True), sink)
    e = jnp.exp(logits - m)
    p = e / (jnp.sum(e, axis=-1, keepdims=True) + jnp.exp(sink - m))
    out = jnp.einsum('bkgnqs,bknsd->bkgnqd', p, v_band)
    return out.reshape(b, hq, s, dh)


def mixing_sublayer(h, w_in, w_out, g_moba, g_sb, g_swa, sinks, rel_bias):
    proj = jnp.einsum('bsd,de->bse', h, w_in)
    sizes = [MOBA_WIDTH] * 3 + [SB_WIDTH] * 3 + [SWA_Q_WIDTH, SWA_KV_WIDTH, SWA_KV_WIDTH]
    cuts = [int(v) for v in np.cumsum(sizes)[:-1]]
    qa, ka, va, qb, kb, vb, qc, kc, vc = jnp.split(proj, cuts, axis=-1)
    o_a = moba_attention(split_heads(qa, MOBA_HEADS), split_heads(ka, MOBA_HEADS), split_heads(va, MOBA_HEADS), rel_bias[:, :MOBA_HEADS])
    o_b = stick_breaking_attention(split_heads(qb, SB_HEADS), split_heads(kb, SB_HEADS), split_heads(vb, SB_HEADS))
    o_c = swa_sink_attention(split_heads(qc, SWA_HEADS), split_heads(kc, SWA_KV_HEADS), split_heads(vc, SWA_KV_HEADS), rel_bias[:, MOBA_HEADS:], sinks)
    o = jnp.concatenate([merge_heads(head_rms_norm(o_a, g_moba)), merge_heads(head_rms_norm(o_b, g_sb)), merge_heads(head_rms_norm(o_c, g_swa))], axis=-1).astype(h.dtype)
    return jnp.einsum('bse,ed->bsd', o, w_out)


def swiglu(h, w_gate, w_up, w_down):
    a = jax.nn.silu(jnp.einsum('bsd,df->bsf', h, w_gate)) * jnp.einsum('bsd,df->bsf', h, w_up)
    return jnp.einsum('bsf,fd->bsd', a, w_down)


def moe_swiglu(h, w_router, w_gate, w_up, w_down):
    logits = jnp.einsum('bsd,de->bse', h.astype(jnp.float32), w_router.astype(jnp.float32))
    top_val, top_idx = lax.top_k(logits, TOP_K)
    top_w = jax.nn.softmax(top_val, axis=-1)
    combine = jnp.sum(jax.nn.one_hot(top_idx, N_EXPERTS, dtype=jnp.float32) * top_w[..., None], axis=-2)
    y = jnp.zeros_like(h)
    for e in range(N_EXPERTS):
        y = y + combine[..., e:e + 1].astype(h.dtype) * swiglu(h, w_gate[e], w_up[e], w_down[e])
    return y


def setup_inputs(seed: int = 0) -> dict:
    key = jax.random.key(seed)
    ks = jax.random.split(key, 22)
    f32 = jnp.float32

    def nrm(k, shape, scale):
        return jax.random.normal(k, shape, f32) * scale

    def gain(k, shape):
        return 1.0 + 0.02 * jax.random.normal(k, shape, f32)

    return {
        'x': nrm(ks[0], (BATCH, SEQ, D_MODEL), 1.0),
        'c': nrm(ks[1], (BATCH, D_MODEL), 1.0),
        'rel_bias': nrm(ks[2], (REL_BUCKETS, N_BIAS_HEADS), 0.5),
        'w_ada': nrm(ks[3], (DEPTH, D_MODEL, ADA_CHUNKS * D_MODEL), 0.5 * D_MODEL ** -0.5),
        'b_ada': nrm(ks[4], (DEPTH, ADA_CHUNKS * D_MODEL), 0.02),
        'g_pre_mix': gain(ks[5], (DEPTH, D_MODEL)),
        'w_in': nrm(ks[6], (DEPTH, D_MODEL, IN_WIDTH), D_MODEL ** -0.5),
        'g_grp_moba': gain(ks[7], (DEPTH, MOBA_WIDTH)),
        'g_grp_sb': gain(ks[8], (DEPTH, SB_WIDTH)),
        'g_grp_swa': gain(ks[9], (DEPTH, SWA_Q_WIDTH)),
        'swa_sinks': nrm(ks[10], (DEPTH, SWA_HEADS), 0.5),
        'w_out': nrm(ks[11], (DEPTH, MIX_WIDTH, D_MODEL), MIX_WIDTH ** -0.5),
        'g_post_mix': gain(ks[12], (DEPTH, D_MODEL)),
        'g_pre_ffn': gain(ks[13], (DEPTH, D_MODEL)),
        'w_ff_gate': nrm(ks[14], (N_DENSE, D_MODEL, D_FF), D_MODEL ** -0.5),
        'w_ff_up': nrm(ks[15], (N_DENSE, D_MODEL, D_FF), D_MODEL ** -0.5),
        'w_ff_down': nrm(ks[16], (N_DENSE, D_FF, D_MODEL), D_FF ** -0.5),
        'w_router': nrm(ks[17], (N_MOE, D_MODEL, N_EXPERTS), D_MODEL ** -0.5),
        'w_moe_gate': nrm(ks[18], (N_MOE, N_EXPERTS, D_MODEL, D_EXPERT), D_MODEL ** -0.5),
        'w_moe_up': nrm(ks[19], (N_MOE, N_EXPERTS, D_MODEL, D_EXPERT), D_MODEL ** -0.5),
        'w_moe_down': nrm(ks[20], (N_MOE, N_EXPERTS, D_EXPERT, D_MODEL), D_EXPERT ** -0.5),
        'g_post_ffn': gain(ks[21], (DEPTH, D_MODEL)),
    }


def reference(x, c, rel_bias, w_ada, b_ada, g_pre_mix, w_in, g_grp_moba, g_grp_sb, g_grp_swa, swa_sinks, w_out, g_post_mix, g_pre_ffn, w_ff_gate, w_ff_up, w_ff_down, w_router, w_moe_gate, w_moe_up, w_moe_down, g_post_ffn):
    for layer in range(DEPTH):
        mod = jnp.einsum('bd,de->be', jax.nn.silu(c), w_ada[layer]) + b_ada[layer]
        shift_m, scale_m, gate_m, shift_f, scale_f, gate_f = jnp.split(mod[:, None, :], ADA_CHUNKS, axis=-1)
        h = rms_norm(x, g_pre_mix[layer]) * (1 + scale_m) + shift_m
        y = mixing_sublayer(h, w_in[layer], w_out[layer], g_grp_moba[layer], g_grp_sb[layer], g_grp_swa[layer], swa_sinks[layer], rel_bias)
        x = x + gate_m * rms_norm(y, g_post_mix[layer])
        h = rms_norm(x, g_pre_ffn[layer]) * (1 + scale_f) + shift_f
        i = layer // 2
        if layer % 2 == 0:
            y = swiglu(h, w_ff_gate[i], w_ff_up[i], w_ff_down[i])
        else:
            y = moe_swiglu(h, w_router[i], w_moe_gate[i], w_moe_up[i], w_moe_down[i])
        x = x + gate_f * rms_norm(y, g_post_ffn[layer])
    return x
```

```python
import ml_dtypes
import numpy as np
from contextlib import ExitStack
import concourse.bass as bass
import concourse.mybir as mybir

F32 = mybir.dt.float32
BF16 = mybir.dt.bfloat16
AF = mybir.ActivationFunctionType
ALU = mybir.AluOpType
AX = mybir.AxisListType

SAME_ENGINE_SYNC = True


class Op:
    __slots__ = ("eng", "fn", "reads", "writes", "dma_key", "signal", "cnt", "waits")

    def __init__(self, eng, fn, reads, writes, dma_key):
        self.eng = eng
        self.fn = fn
        self.reads = reads
        self.writes = writes
        self.dma_key = dma_key
        self.signal = False
        self.cnt = 0
        self.waits = None


class Prog:
    ENGS = ("pe", "act", "dve", "pool", "sp")

    def __init__(self, nc):
        self.nc = nc
        self.ops = []
        self.ctx = ExitStack()
        self._n = 0
        self.excl = set()

    def sb(self, shape, dt, name=None):
        self._n += 1
        return self.ctx.enter_context(self.nc.sbuf_tensor(name or f"sb{self._n}", list(shape), dt))

    def ps(self, shape, dt, name=None):
        self._n += 1
        if name:
            self.excl.add(name)
        return self.ctx.enter_context(self.nc.psum_tensor(name or f"ps{self._n}", list(shape), dt))

    def op(self, eng, fn, reads=(), writes=()):
        self.ops.append(Op(eng, fn, tuple(reads), tuple(writes), None))

    def dma(self, eng, out, in_, reads=(), writes=(), key=None):
        if key is None:
            key = (tuple(writes) + tuple(reads))[0]
        self.ops.append(Op(eng, lambda e: e.dma_start(out=out, in_=in_), tuple(reads), tuple(writes), "dma:" + str(key)))

    def emit(self):
        nc = self.nc
        ops = self.ops
        last_writer = {}
        readers = {}
        deps = [None] * len(ops)
        for i, op in enumerate(ops):
            d = set()
            for k in op.reads:
                if k in last_writer:
                    d.add(last_writer[k])
                if k in self.excl:
                    for r in readers.get(k, ()):
                        if ops[r].eng != op.eng:
                            d.add(r)
            for k in op.writes:
                if k in last_writer:
                    d.add(last_writer[k])
                for r in readers.get(k, ()):
                    d.add(r)
            for k in op.reads:
                readers.setdefault(k, []).append(i)
            for k in op.writes:
                last_writer[k] = i
                readers[k] = []
            d.discard(i)
            deps[i] = d
        for i, op in enumerate(ops):
            for j in deps[i]:
                pj = ops[j]
                if pj.dma_key is not None:
                    pj.signal = True
                elif pj.eng != op.eng or (SAME_ENGINE_SYNC and op.eng != "pe"):
                    pj.signal = True
        eng_cnt = {e: 0 for e in self.ENGS}
        dma_cnt = {}
        for op in ops:
            if op.dma_key is not None:
                dma_cnt[op.dma_key] = dma_cnt.get(op.dma_key, 0) + 16
                op.cnt = dma_cnt[op.dma_key]
                op.signal = True
            elif op.signal:
                eng_cnt[op.eng] += 1
                op.cnt = eng_cnt[op.eng]
        sems = {}
        for e in self.ENGS:
            sems["eng:" + e] = self.ctx.enter_context(nc.semaphore("s_" + e))
        for n, k in enumerate(sorted(dma_cnt)):
            sems[k] = self.ctx.enter_context(nc.semaphore(f"s_dma{n}"))
        self.n_sems = len(sems)
        waited = {e: {} for e in self.ENGS}
        for i, op in enumerate(ops):
            need = {}
            for j in deps[i]:
                pj = ops[j]
                if pj.dma_key is not None:
                    sk = pj.dma_key
                elif pj.eng != op.eng or (SAME_ENGINE_SYNC and op.eng != "pe"):
                    sk = "eng:" + pj.eng
                else:
                    continue
                if pj.cnt > need.get(sk, 0):
                    need[sk] = pj.cnt
            w = []
            for sk, v in need.items():
                if waited[op.eng].get(sk, 0) < v:
                    waited[op.eng][sk] = v
                    w.append((sk, v))
            op.waits = w
        final_waits = [(k, v) for k, v in dma_cnt.items()]
        per_eng = {e: [op for op in ops if op.eng == e] for e in self.ENGS}
        block = self.ctx.enter_context(nc.Block())

        def run(e_name, eng):
            for op in per_eng[e_name]:
                for sk, v in op.waits:
                    eng.wait_ge(sems[sk], v)
                ins = op.fn(eng)
                if op.signal:
                    if op.dma_key is not None:
                        ins.then_inc(sems[op.dma_key], 16)
                    else:
                        ins.then_inc(sems["eng:" + e_name], 1)
            if e_name == "sp":
                for sk, v in final_waits:
                    eng.wait_ge(sems[sk], v)

        @block.tensor
        def _(e):
            run("pe", e)

        @block.scalar
        def _(e):
            run("act", e)

        @block.vector
        def _(e):
            run("dve", e)

        @block.gpsimd
        def _(e):
            run("pool", e)

        @block.sync
        def _(e):
            run("sp", e)

        self.ctx.close()
        return nc


D = 4096
KC = 32
NEG = -30000.0
N_TAB = 128 * 256 + 2 * 128 * 512
N_TAB_CORE = N_TAB // 8
ADA_CORE = 24576 // 8


def rel_bucket_np(dist):
    import math
    n = np.maximum(dist, 0)
    nf = np.maximum(n, 1).astype(np.float32)
    v = (np.log(nf / np.float32(16)) / np.float32(math.log(128 / 16)) * np.float32(16)).astype(np.float32)
    large = 16 + v.astype(np.int32)
    return np.where(n < 16, n, np.minimum(large, 31)).astype(np.int64)


def build_onehot():
    p = np.arange(128)[:, None]
    cols = []
    j = np.arange(256)[None, :]
    dist = p + 128 - j
    valid = (dist >= 0) & (dist < 128)
    cols.append((np.where(valid, rel_bucket_np(dist), 32)).reshape(-1))
    for qpos in range(2):
        dprev = 256 + qpos * 128 + p - j
        down = qpos * 128 + p - j
        blk = np.concatenate([rel_bucket_np(dprev), np.where(down >= 0, rel_bucket_np(down), 32)], axis=1)
        cols.append(blk.reshape(-1))
    idx = np.concatenate(cols)
    oh = np.zeros((33, N_TAB), np.float32)
    oh[idx, np.arange(N_TAB)] = 1.0
    return oh


def build_k0():
    nc = bass.Bass("TRN2", target_bir_lowering=False)
    P = Prog(nc)
    dr = lambda n, sh, dt, kind="ExternalInput": nc.dram_tensor(n, list(sh), dt, kind=kind).ap()
    cT = dr("cT", [128, KC * 2], F32)
    wada = dr("wada", [12, 128, KC * 512], F32)
    bada = dr("bada", [2, 2 * ADA_CORE], F32)
    rbaug = dr("rbaug", [33, 24], F32)
    oh = dr("oh", [33, N_TAB_CORE], F32)
    modo = dr("modo", [2, 2, ADA_CORE], F32, kind="ExternalOutput")
    tabo = dr("tabo", [24, N_TAB_CORE], F32, kind="ExternalOutput")

    cs = P.sb([128, KC, 2], F32, "cs")
    wb = [P.sb([128, KC, 512], F32, f"wb{i}") for i in range(2)]
    bsb = P.sb([2, 2 * ADA_CORE], F32, "bsb")
    rb = P.sb([33, 24], F32, "rb")
    ohs = [P.sb([33, 2048], F32, f"ohs{i}") for i in range(2)]
    mo = [P.sb([2, 512], F32, f"mo{i}") for i in range(2)]
    to = [P.sb([24, 512], F32, f"to{i}") for i in range(2)]
    pss = [P.ps([128, 512], F32, f"ps{i}") for i in range(4)]

    P.dma("sp", cs[:], cT.rearrange("p (kc b) -> p kc b", b=2), writes=["cs"])
    P.dma("sp", bsb[:], bada, writes=["bsb"])
    P.dma("sp", rb[:], rbaug, writes=["rb"])
    P.op("act", lambda e: e.activation(out=cs[:], in_=cs[:], func=AF.Silu), reads=["cs"], writes=["cs"])
    n = 0
    for l in range(2):
        for g in range(6):
            b = n % 2
            P.dma("sp", wb[b][:], wada[l * 6 + g].rearrange("p (kc n) -> p kc n", n=512), writes=[f"wb{b}"])
            for kc in range(KC):
                P.op("pe", lambda e, kc=kc, b=b: e.matmul(pss[b][0:2, :], cs[:, kc, :], wb[b][:, kc, :], start=(kc == 0), stop=(kc == KC - 1)),
                     reads=["cs", f"wb{b}"], writes=[f"ps{b}"])
            P.op("dve", lambda e, b=b, l=l, g=g: e.tensor_tensor(out=mo[b][:], in0=pss[b][0:2, :],
                                                                in1=bsb[:, l * ADA_CORE + g * 512:l * ADA_CORE + (g + 1) * 512], op=ALU.add),
                 reads=[f"ps{b}", "bsb"], writes=[f"mo{b}"])
            P.dma("sp", modo[l, :, g * 512:(g + 1) * 512], mo[b][:], reads=[f"mo{b}"], key=f"moo{b}")
            n += 1
    for t in range(N_TAB_CORE // 512):
        b = t % 2
        ob = (t // 4) % 2
        if t % 4 == 0:
            P.dma("sp", ohs[ob][:], oh[:, t * 512:t * 512 + 2048], writes=[f"ohs{ob}"])
        P.op("pe", lambda e, t=t, b=b, ob=ob: e.matmul(pss[2 + b][0:24, :], rb[:], ohs[ob][:, (t % 4) * 512:(t % 4 + 1) * 512], start=True, stop=True),
             reads=["rb", f"ohs{ob}"], writes=[f"ps{2 + b}"])
        P.op("act", lambda e, b=b: e.activation(out=to[b][:], in_=pss[2 + b][0:24, :], func=AF.Copy), reads=[f"ps{2 + b}"], writes=[f"to{b}"])
        P.dma("sp", tabo[:, t * 512:(t + 1) * 512], to[b][:], reads=[f"to{b}"], key=f"too{b}")
    return P.emit()


D = 4096
KC = 32
TOK = 1024
IN_W = 8704
FM_CHUNKS = list(range(0, 16)) + list(range(24, 40)) + list(range(48, 66))
Q_CHUNKS = set(list(range(0, 8)) + list(range(24, 32)) + list(range(48, 64)))
V_COLS = list(range(16 * 128, 24 * 128)) + list(range(40 * 128, 48 * 128)) + list(range(66 * 128, 68 * 128))
V_GROUPS = [(0, 512), (512, 512), (1024, 512), (1536, 512), (2048, 256)]
NV = 2304
QSCALE = 128 ** -0.5
EPS = 1e-6


def build_k1():
    nc = bass.Bass("TRN2", target_bir_lowering=False)
    P = Prog(nc)
    xT = nc.dram_tensor("xT", [D, TOK], F32, kind="ExternalInput").ap()
    vecs = nc.dram_tensor("vecs", [128, 3 * KC], F32, kind="ExternalInput").ap()
    wfm = nc.dram_tensor("wfm", [50, 128, KC * 128], F32, kind="ExternalInput").ap()
    wv = nc.dram_tensor("wv", [128, KC * NV], F32, kind="ExternalInput").ap()
    outT = nc.dram_tensor("outT", [50, 128, TOK], BF16, kind="ExternalOutput").ap()
    outV = nc.dram_tensor("outV", [TOK, NV], BF16, kind="ExternalOutput").ap()

    ones = P.sb([128, 128], F32, "ones")
    vec = P.sb([128, 3 * KC], F32, "vec")
    gs = P.sb([128, KC], F32, "gs")
    h = P.sb([128, KC, TOK], BF16, "h")
    NT = 128
    NTT = TOK // NT
    xb = [P.sb([128, KC, NT], F32, f"xb{i}") for i in range(2)]
    sq = P.sb([128, KC, NT], F32, "sq")
    lnt = P.sb([128, NT], F32, "lnt")
    rstd = P.sb([128, NT], F32, "rstd")
    t1 = [P.sb([128, NT], F32, f"t1_{i}") for i in range(2)]
    wb = [P.sb([128, KC, 128], BF16, f"wb{i}") for i in range(2)]
    wvb = [P.sb([128, KC, 512], BF16, f"wvb{i}") for i in range(2)]
    stg = [P.sb([128, TOK], BF16, f"stg{i}") for i in range(2)]
    stv = [P.sb([128, 512], BF16, f"stv{i}") for i in range(2)]
    pss = [P.ps([128, 512], F32, f"psb{i}") for i in range(8)]

    P.op("pool", lambda e: e.memset(ones[:], 1.0), writes=["ones"])
    P.dma("sp", vec[:], vecs, writes=["vec"])
    P.op("dve", lambda e: e.tensor_scalar(out=gs[:], in0=vec[:, KC:2 * KC], scalar1=1.0, scalar2=None, op0=ALU.add),
         reads=["vec"], writes=["gs"])
    P.op("dve", lambda e: e.tensor_tensor(out=gs[:], in0=gs[:], in1=vec[:, 0:KC], op=ALU.mult),
         reads=["vec", "gs"], writes=["gs"])

    xT_v = xT.rearrange("(kc p) t -> p kc t", p=128)
    for tt in range(NTT):
        b = tt % 2
        P.dma("sp", xb[b][:], xT_v[:, :, tt * NT:(tt + 1) * NT], writes=[f"xb{b}"])
        P.op("act", lambda e, b=b: e.activation(out=sq[:], in_=xb[b][:], func=AF.Square),
             reads=[f"xb{b}"], writes=["sq"])
        for kc in range(KC):
            P.op("pe", lambda e, kc=kc: e.matmul(pss[0][:, 0:NT], ones[:], sq[:, kc, :], start=(kc == 0), stop=(kc == KC - 1)),
                 reads=["ones", "sq"], writes=["ps0"])
        P.op("act", lambda e: e.activation(out=lnt[:], in_=pss[0][:, 0:NT], func=AF.Ln, scale=1.0 / D, bias=EPS),
             reads=["ps0"], writes=["lnt"])
        P.op("act", lambda e: e.activation(out=rstd[:], in_=lnt[:], func=AF.Exp, scale=-0.5),
             reads=["lnt"], writes=["rstd"])
        for kc in range(KC):
            tb = kc % 2
            P.op("dve", lambda e, kc=kc, tb=tb, b=b: e.scalar_tensor_tensor(
                out=t1[tb][:], in0=xb[b][:, kc, :], scalar=gs[:, kc:kc + 1], in1=rstd[:], op0=ALU.mult, op1=ALU.mult),
                reads=[f"xb{b}", "gs", "rstd"], writes=[f"t1_{tb}"])
            P.op("act", lambda e, kc=kc, tb=tb, tt=tt: e.activation(
                out=h[:, kc, tt * NT:(tt + 1) * NT], in_=t1[tb][:], func=AF.Identity, bias=vec[:, 2 * KC + kc:2 * KC + kc + 1]),
                reads=[f"t1_{tb}", "vec"], writes=[f"h{tt}"])
    hkeys = [f"h{tt}" for tt in range(NTT)]

    n_ps = 0
    for ci in range(50):
        c = FM_CHUNKS[ci]
        b = ci % 2
        P.dma("pool", wb[b][:], wfm[ci].rearrange("p (kc n) -> p kc n", n=128), writes=[f"wb{b}"])
        for t2 in range(2):
            pb = 2 + (n_ps % 4)
            n_ps += 1
            for kc in range(KC):
                P.op("pe", lambda e, kc=kc, b=b, t2=t2, pb=pb: e.matmul(
                    pss[pb][:], wb[b][:, kc, :], h[:, kc, t2 * 512:(t2 + 1) * 512], start=(kc == 0), stop=(kc == KC - 1)),
                    reads=[f"wb{b}"] + hkeys[4 * t2:4 * t2 + 4], writes=[f"ps{pb}"])
            sc = QSCALE if c in Q_CHUNKS else 1.0
            if n_ps % 2 == 0:
                P.op("act", lambda e, b=b, t2=t2, pb=pb, sc=sc: e.activation(
                    out=stg[b][:, t2 * 512:(t2 + 1) * 512], in_=pss[pb][:], func=AF.Copy, scale=sc),
                    reads=[f"ps{pb}"], writes=[f"stg{b}"])
            else:
                P.op("dve", lambda e, b=b, t2=t2, pb=pb, sc=sc: e.tensor_scalar(
                    out=stg[b][:, t2 * 512:(t2 + 1) * 512], in0=pss[pb][:], scalar1=sc, scalar2=None, op0=ALU.mult),
                    reads=[f"ps{pb}"], writes=[f"stg{b}"])
        P.dma("sp", outT[ci], stg[b][:], reads=[f"stg{b}"], key=f"stgo{b}")

    wv_off = 0
    nst = 0
    for gi, (c0, wdt) in enumerate(V_GROUPS):
        b = gi % 2
        P.dma("pool", wvb[b][:, :, 0:wdt], wv[:, wv_off:wv_off + KC * wdt].rearrange("p (kc n) -> p kc n", n=wdt),
              writes=[f"wvb{b}"])
        wv_off += KC * wdt
        for t8 in range(8):
            pb = 2 + (n_ps % 4)
            n_ps += 1
            for kc in range(KC):
                P.op("pe", lambda e, kc=kc, b=b, t8=t8, pb=pb, wdt=wdt: e.matmul(
                    pss[pb][:, 0:wdt], h[:, kc, t8 * 128:(t8 + 1) * 128], wvb[b][:, kc, 0:wdt], start=(kc == 0), stop=(kc == KC - 1)),
                    reads=[f"wvb{b}", hkeys[t8]], writes=[f"ps{pb}"])
            sb_ = nst % 2
            nst += 1
            if nst % 2 == 0:
                P.op("act", lambda e, sb_=sb_, pb=pb, wdt=wdt: e.activation(out=stv[sb_][:, 0:wdt], in_=pss[pb][:, 0:wdt], func=AF.Copy),
                     reads=[f"ps{pb}"], writes=[f"stv{sb_}"])
            else:
                P.op("dve", lambda e, sb_=sb_, pb=pb, wdt=wdt: e.tensor_copy(out=stv[sb_][:, 0:wdt], in_=pss[pb][:, 0:wdt]),
                     reads=[f"ps{pb}"], writes=[f"stv{sb_}"])
            P.dma("sp", outV[t8 * 128:(t8 + 1) * 128, c0:c0 + wdt], stv[sb_][:, 0:wdt], reads=[f"stv{sb_}"], key=f"stvo{sb_}")
    return P.emit()


def k1_host_weights(w_in_l):
    w = w_in_l.reshape(KC, 128, IN_W)
    wfm = np.empty((50, 128, KC * 128), np.float32)
    for ci, c in enumerate(FM_CHUNKS):
        wfm[ci] = w[:, :, c * 128:(c + 1) * 128].transpose(1, 0, 2).reshape(128, KC * 128)
    wvv = w[:, :, V_COLS]
    parts = []
    for (c0, wdt) in V_GROUPS:
        parts.append(wvv[:, :, c0:c0 + wdt].transpose(1, 0, 2).reshape(128, KC * wdt))
    wv = np.concatenate(parts, axis=1)
    return wfm, np.ascontiguousarray(wv)


S = 4096
NEG = -30000.0
EPS = 1e-6
HD = 128


def build_k2(parts=("moba", "sb", "swa"), nq_sb=8, nt_tok=32, nheads=None):
    nc = bass.Bass("TRN2", target_bir_lowering=False)
    P = Prog(nc)
    dr = lambda n, sh, dt, kind="ExternalInput": nc.dram_tensor(n, list(sh), dt, kind=kind).ap()
    qk = dr("qk", [13, 128, S], BF16)
    vv = dr("vv", [5, S, HD], BF16)
    mbias = dr("mbias", [2, 2, 128, 512], F32)
    mtab31 = dr("mtab31", [128, 2], F32)
    wbias = dr("wbias", [4, 128, 256], F32)
    sinks = dr("sinks", [128, 4], F32)
    gq = dr("gq", [6, 128, 128], F32)
    gp = dr("gp", [128, 2], F32)
    c_ident = dr("c_ident", [128, 128], BF16)
    c_f32 = dr("c_f32", [3, 128, 128], F32)
    c_sbmask = dr("c_sbmask", [128, 4 * 512], F32)
    oT = dr("oT", [8, 128, S], BF16, kind="ExternalOutput")

    ident = P.sb([128, 128], BF16, "ident")
    cf = P.sb([128, 3, 128], F32, "cf")
    sbmask = P.sb([128, 4, 512], F32, "sbmask")
    qb_ = [P.sb([128, S], BF16, f"qb{i}") for i in range(2)]
    kb_ = [P.sb([128, S], BF16, f"kb{i}") for i in range(2)]
    vb_ = [P.sb([128, 32, HD], BF16, f"vb{i}") for i in range(2)]
    ost = [P.sb([128, S], BF16, f"ost{i}") for i in range(2)]
    mb_sb = P.sb([128, 2, 2, 512], F32, "mb_sb")
    mt31 = P.sb([128, 2], F32, "mt31")
    wb_sb = P.sb([128, 4, 256], F32, "wb_sb")
    sink_sb = P.sb([128, 4], F32, "sink_sb")
    gq_sb = P.sb([128, 6, 128], F32, "gq_sb")
    gp_sb = P.sb([128, 2], F32, "gp_sb")
    pA = [P.ps([128, 512], F32, f"pA{i}") for i in range(2)]
    pZ = [P.ps([128, 512], F32, f"pZ{i}") for i in range(2)]
    pO = P.ps([128, 512], F32, "pO")
    pG = P.ps([128, 512], F32, "pG")
    pTall = P.ps([128, 2, 512], BF16, "pTall")
    pTs = [pTall[:, 0, :], pTall[:, 1, :]]
    P.excl.update(["pTs0", "pTs1"])
    pTh = P.ps([128, 128], BF16, "pTh")

    P.dma("sp", ident[:], c_ident, writes=["ident"])
    P.dma("sp", cf[:], c_f32.rearrange("c p n -> p c n"), writes=["cf"])
    P.dma("sp", sbmask[:], c_sbmask.rearrange("p (o f) -> p o f", o=4), writes=["sbmask"])
    P.dma("sp", mb_sb[:], mbias.rearrange("h q p f -> p h q f"), writes=["mb_sb"])
    P.dma("sp", mt31[:], mtab31, writes=["mt31"])
    P.dma("sp", wb_sb[:], wbias.rearrange("h p f -> p h f"), writes=["wb_sb"])
    P.dma("sp", sink_sb[:], sinks, writes=["sink_sb"])
    P.dma("sp", gq_sb[:], gq.rearrange("h p f -> p h f"), writes=["gq_sb"])
    P.dma("sp", gp_sb[:], gp, writes=["gp_sb"])
    negLge = cf[:, 0, :]
    negones = cf[:, 1, :]
    ones = cf[:, 2, :]

    state = {"nload": 0, "nost": 0}

    def load_qkv(qi, ki, vi):
        b = state["nload"] % 2
        state["nload"] += 1
        if qi is not None:
            P.dma("sp", qb_[b][:], qk[qi], writes=[f"qb{b}"])
        if ki is not None:
            P.dma("sp", kb_[b][:], qk[ki], writes=[f"kb{b}"])
        if vi is not None:
            P.dma("sp", vb_[b][:], vv[vi].rearrange("(t p) d -> p t d", p=128), writes=[f"vb{b}"])
        return b

    sc = {}

    def scr(name, shape, dt):
        if name not in sc:
            sc[name] = P.sb(shape, dt, name)
        return sc[name]

    def head_norm_tok(o_ps, o_key, rden, gidx, ob, q0, tagn):
        on = scr("hn_on", [128, 128], F32)
        junk = scr("hn_junk", [128, 128], F32)
        ss = scr("hn_ss", [128, 1], F32)
        lt = scr("hn_lt", [128, 1], F32)
        rs = scr("hn_rs", [128, 1], F32)
        ob16 = scr("hn_ob16", [128, 128], BF16)
        P.op("dve", lambda e: e.tensor_scalar(out=on[:], in0=o_ps, scalar1=rden, scalar2=None, op0=ALU.mult),
             reads=[o_key, "rden"], writes=["hn_on"])
        P.op("act", lambda e: e.activation(out=junk[:], in_=on[:], func=AF.Square, accum_out=ss[:]),
             reads=["hn_on"], writes=["hn_junk", "hn_ss"])
        P.op("act", lambda e: e.activation(out=lt[:], in_=ss[:], func=AF.Ln, scale=1.0 / HD, bias=EPS),
             reads=["hn_ss"], writes=["hn_lt"])
        P.op("act", lambda e: e.activation(out=rs[:], in_=lt[:], func=AF.Exp, scale=-0.5),
             reads=["hn_lt"], writes=["hn_rs"])
        P.op("dve", lambda e: e.scalar_tensor_tensor(out=ob16[:], in0=on[:], scalar=rs[:], in1=gq_sb[:, gidx, :],
                                                     op0=ALU.mult, op1=ALU.mult),
             reads=["hn_on", "hn_rs", "gq_sb"], writes=["hn_ob16"])
        P.op("pe", lambda e: e.transpose(pTh[:], ob16[:], ident[:]),
             reads=["hn_ob16", "ident"], writes=["pTh"])
        P.op("act", lambda e: e.activation(out=ost[ob][:, q0:q0 + 128], in_=pTh[:], func=AF.Copy),
             reads=["pTh"], writes=[f"ost{ob}"])

    out_idx = 0
    if "moba" in parts:
        for hh in range(2):
            b = load_qkv(2 * hh, 2 * hh + 1, hh)
            q, k, v = qb_[b], kb_[b], vb_[b]
            kq, kk, kv = f"qb{b}", f"kb{b}", f"vb{b}"
            ob = state["nost"] % 2
            state["nost"] += 1
            kms = scr("kms", [128, 16], F32)
            kmean = scr("kmean", [128, 16], BF16)
            P.op("dve", lambda e, k=k: e.tensor_reduce(out=kms[:], in_=k[:].rearrange("p (n j) -> p n j", j=256), axis=AX.X, op=ALU.add),
                 reads=[kk], writes=["kms"])
            P.op("dve", lambda e: e.tensor_scalar(out=kmean[:], in0=kms[:], scalar1=1.0 / 256, scalar2=None, op0=ALU.mult),
                 reads=["kms"], writes=["kmean"])
            srow = scr("srow", [128, S], F32)
            prow = scr("prow", [128, S], BF16)
            gate = scr("gate", [128, 16], F32)
            max8 = scr("max8", [128, 8], F32)
            selb = scr("selb", [128, 16], F32)
            cb = scr("cb", [128, 16], F32)
            rmax = scr("rmax", [128, 1], F32)
            rsum = scr("rsum", [128, 1], F32)
            rden = scr("rden", [128, 1], F32)
            pts = [scr(f"pts{i}", [128, 512], BF16) for i in range(2)]
            for i in range(nt_tok):
                qbk, qpos = i // 2, i % 2
                q0 = i * 128
                own_w = 128 if qpos == 0 else 256
                L = qbk * 256 + own_w
                qt = q[:, q0:q0 + 128]
                if qbk >= 4:
                    P.op("pe", lambda e, qt=qt: e.matmul(pG[:, 0:16], qt, kmean[:], start=True, stop=True),
                         reads=[kq, "kmean"], writes=["pG"])
                    P.op("dve", lambda e: e.memset(gate[:], -1e30), writes=["gate"])
                    P.op("dve", lambda e, qbk=qbk: e.tensor_copy(out=gate[:, 0:qbk], in_=pG[:, 0:qbk]),
                         reads=["pG"], writes=["gate"])
                    P.op("dve", lambda e: e.max(out=max8[:], in_=gate[:]), reads=["gate"], writes=["max8"])
                    P.op("dve", lambda e: e.tensor_scalar(out=selb[:], in0=gate[:], scalar1=max8[:, 2:3], scalar2=NEG,
                                                          op0=ALU.is_lt, op1=ALU.mult),
                         reads=["gate", "max8"], writes=["selb"])
                else:
                    P.op("dve", lambda e: e.memset(selb[:], 0.0), writes=["selb"])
                P.op("dve", lambda e, hh=hh: e.tensor_scalar(out=cb[:], in0=selb[:], scalar1=mt31[:, hh:hh + 1], scalar2=None, op0=ALU.add),
                     reads=["selb", "mt31"], writes=["cb"])
                nblk = qbk + 1
                for m in range((nblk + 1) // 2):
                    pb = m % 2
                    n0 = 2 * m
                    wcols = min(512, L - n0 * 256)
                    P.op("pe", lambda e, qt=qt, pb=pb, n0=n0, wcols=wcols, k=k: e.matmul(
                        pA[pb][:, 0:wcols], qt, k[:, n0 * 256:n0 * 256 + wcols], start=True, stop=True),
                        reads=[kq, kk], writes=[f"pA{pb}"])
                    for n in (n0, n0 + 1):
                        if n > qbk:
                            continue
                        c0 = (n - n0) * 256
                        if n < qbk - 1:
                            P.op("dve", lambda e, pb=pb, c0=c0, n=n: e.tensor_scalar(
                                out=srow[:, n * 256:(n + 1) * 256], in0=pA[pb][:, c0:c0 + 256], scalar1=cb[:, n:n + 1], scalar2=None, op0=ALU.add),
                                reads=[f"pA{pb}", "cb"], writes=["srow"])
                        elif n == qbk - 1:
                            P.op("dve", lambda e, pb=pb, c0=c0, n=n, hh=hh, qpos=qpos: e.scalar_tensor_tensor(
                                out=srow[:, n * 256:(n + 1) * 256], in0=pA[pb][:, c0:c0 + 256], scalar=selb[:, n:n + 1],
                                in1=mb_sb[:, hh, qpos, 0:256], op0=ALU.add, op1=ALU.add),
                                reads=[f"pA{pb}", "selb", "mb_sb"], writes=["srow"])
                        else:
                            P.op("dve", lambda e, pb=pb, c0=c0, n=n, hh=hh, qpos=qpos, own_w=own_w: e.tensor_tensor(
                                out=srow[:, n * 256:n * 256 + own_w], in0=pA[pb][:, c0:c0 + own_w],
                                in1=mb_sb[:, hh, qpos, 256:256 + own_w], op=ALU.add),
                                reads=[f"pA{pb}", "mb_sb"], writes=["srow"])
                P.op("dve", lambda e, L=L: e.tensor_reduce(out=rmax[:], in_=srow[:, 0:L], axis=AX.X, op=ALU.max),
                     reads=["srow"], writes=["rmax"])
                P.op("dve", lambda e: e.tensor_scalar(out=rmax[:], in0=rmax[:], scalar1=-1.0, scalar2=None, op0=ALU.mult),
                     reads=["rmax"], writes=["rmax"])
                P.op("act", lambda e, L=L: e.activation(out=prow[:, 0:L], in_=srow[:, 0:L], func=AF.Exp, bias=rmax[:], accum_out=rsum[:]),
                     reads=["srow", "rmax"], writes=["prow", "rsum"])
                P.op("dve", lambda e: e.reciprocal(out=rden[:], in_=rsum[:]), reads=["rsum"], writes=["rden"])
                nkt = L // 128
                for g0 in range(0, nkt, 4):
                    gb = (g0 // 4) % 2
                    ng = min(4, nkt - g0)
                    for j in range(ng):
                        kt = g0 + j
                        P.op("pe", lambda e, gb=gb, j=j, kt=kt: e.transpose(pTs[gb][:, j * 128:(j + 1) * 128], prow[:, kt * 128:(kt + 1) * 128], ident[:]),
                             reads=["prow", "ident"], writes=[f"pTs{gb}"])
                    if (g0 // 4) % 2 == 0:
                        P.op("act", lambda e, gb=gb, ng=ng: e.activation(out=pts[gb][:, 0:ng * 128], in_=pTs[gb][:, 0:ng * 128], func=AF.Copy),
                             reads=[f"pTs{gb}"], writes=[f"pts{gb}"])
                    else:
                        P.op("dve", lambda e, gb=gb, ng=ng: e.tensor_copy(out=pts[gb][:, 0:ng * 128], in_=pTs[gb][:, 0:ng * 128]),
                             reads=[f"pTs{gb}"], writes=[f"pts{gb}"])
                    for j in range(ng):
                        kt = g0 + j
                        P.op("pe", lambda e, gb=gb, j=j, kt=kt, nkt=nkt, v=v: e.matmul(
                            pO[:, 0:128], pts[gb][:, j * 128:(j + 1) * 128], v[:, kt, :], start=(kt == 0), stop=(kt == nkt - 1)),
                            reads=[f"pts{gb}", kv], writes=["pO"])
                head_norm_tok(pO[:, 0:128], "pO", rden[:], hh, ob, q0, "m")
            P.dma("sp", oT[out_idx], ost[ob][:], reads=[f"ost{ob}"], key=f"osto{ob}")
            out_idx += 1
    else:
        out_idx = 2

    if "sb" in parts:
        for hh in range(2):
            b = load_qkv(4 + 2 * hh, 5 + 2 * hh, 2 + hh)
            q, k, v = qb_[b], kb_[b], vb_[b]
            kq, kk, kv = f"qb{b}", f"kb{b}", f"vb{b}"
            ob = state["nost"] % 2
            state["nost"] += 1
            ees = [scr(f"sb_e{i}", [128, 512], F32) for i in range(2)]
            azs = [scr(f"sb_az{i}", [128, 512], F32) for i in range(2)]
            spb = [scr(f"sb_sp{i}", [128, 512], F32) for i in range(2)]
            spsum = scr("sb_spsum", [128, 512], F32)
            a32s = [scr(f"sb_a32{i}", [128, 512], F32) for i in range(2)]
            aT = [scr(f"sb_aT{i}", [128, 512], BF16) for i in range(2)]
            osq = scr("sb_osq", [128, 512], F32)
            o32 = scr("sb_o32", [128, 512], F32)
            lnt = scr("sb_lnt", [128, 512], F32)
            rst = scr("sb_rst", [128, 512], F32)
            npair = 0
            for qi in range(nq_sb):
                Q0 = qi * 512
                qt = q[:, Q0:Q0 + 512]
                Jtop = 4 * qi + 3
                fronts, backs = [], []
                for J in range(Jtop, -1, -1):
                    o = J - 4 * qi
                    _mark0 = len(P.ops)
                    pb = npair % 2
                    npair += 1
                    first = (J == Jtop)
                    sp = spb[pb]
                    ksp = f"sb_sp{pb}"
                    ee, az, a32 = ees[pb], azs[pb], a32s[pb]
                    kee, kaz, ka32 = f"sb_e{pb}", f"sb_az{pb}", f"sb_a32{pb}"
                    P.op("pe", lambda e, pb=pb, J=J, qt=qt, k=k: e.matmul(pZ[pb][:], k[:, J * 128:(J + 1) * 128], qt, start=True, stop=True),
                         reads=[kq, kk], writes=[f"pZ{pb}"])
                    P.op("dve", lambda e, pb=pb, az=az: e.tensor_scalar(out=az[:], in0=pZ[pb][:], scalar1=40.0, scalar2=None, op0=ALU.min),
                         reads=[f"pZ{pb}"], writes=[kaz])
                    P.op("act", lambda e, ee=ee, az=az: e.activation(out=ee[:], in_=az[:], func=AF.Exp),
                         reads=[kaz], writes=[kee])
                    P.op("act", lambda e, ee=ee: e.activation(out=ee[:], in_=ee[:], func=AF.Ln, bias=1.0),
                         reads=[kee], writes=[kee])
                    P.op("dve", lambda e, pb=pb, sp=sp, ee=ee: e.tensor_tensor(out=sp[:], in0=pZ[pb][:], in1=ee[:], op=ALU.max),
                         reads=[f"pZ{pb}", kee], writes=[ksp])
                    if o >= 0:
                        P.op("dve", lambda e, sp=sp, o=o: e.tensor_tensor(out=sp[:], in0=sp[:], in1=sbmask[:, o, :], op=ALU.mult),
                             reads=[ksp, "sbmask"], writes=[ksp])
                    _mark1 = len(P.ops)
                    P.op("pe", lambda e, pb=pb, J=J, qt=qt, k=k: e.matmul(pA[pb][:], k[:, J * 128:(J + 1) * 128], qt, start=True, stop=False),
                         reads=[kq, kk], writes=[f"pA{pb}"])
                    if True:
                        P.op("pe", lambda e, pb=pb, sp=sp, first=first: e.matmul(pA[pb][:], negLge, sp[:], start=False, stop=first),
                             reads=[ksp, "cf"], writes=[f"pA{pb}"])
                        if not first:
                            P.op("pe", lambda e, pb=pb: e.matmul(pA[pb][:], negones, spsum[:], start=False, stop=True),
                                 reads=["sb_spsum", "cf"], writes=[f"pA{pb}"])
                    if o >= 0:
                        P.op("act", lambda e, pb=pb, a32=a32: e.activation(out=a32[:], in_=pA[pb][:], func=AF.Exp),
                             reads=[f"pA{pb}"], writes=[ka32])
                        P.op("dve", lambda e, pb=pb, o=o, a32=a32: e.tensor_tensor(out=aT[pb][:], in0=a32[:], in1=sbmask[:, o, :], op=ALU.mult),
                             reads=[ka32, "sbmask"], writes=[f"sb_aT{pb}"])
                    else:
                        P.op("act", lambda e, pb=pb: e.activation(out=aT[pb][:], in_=pA[pb][:], func=AF.Exp),
                             reads=[f"pA{pb}"], writes=[f"sb_aT{pb}"])
                    P.op("pe", lambda e, pb=pb, J=J, Jtop=Jtop, v=v: e.matmul(pO[:], v[:, J, :], aT[pb][:], start=(J == Jtop), stop=(J == 0)),
                         reads=[f"sb_aT{pb}", kv], writes=["pO"])
                    if J > 0:
                        if first:
                            P.op("dve", lambda e, sp=sp: e.tensor_copy(out=spsum[:], in_=sp[:]),
                                 reads=[ksp], writes=["sb_spsum"])
                        else:
                            P.op("dve", lambda e, sp=sp: e.tensor_tensor(out=spsum[:], in0=spsum[:], in1=sp[:], op=ALU.add),
                                 reads=[ksp, "sb_spsum"], writes=["sb_spsum"])
                    fronts.append(P.ops[_mark0:_mark1])
                    backs.append(P.ops[_mark1:])
                    del P.ops[_mark0:]
                P.ops.extend(fronts[0])
                for n_ in range(len(fronts)):
                    if n_ + 1 < len(fronts):
                        P.ops.extend(fronts[n_ + 1])
                    P.ops.extend(backs[n_])
                P.op("dve", lambda e: e.tensor_copy(out=o32[:], in_=pO[:]), reads=["pO"], writes=["sb_o32"])
                P.op("act", lambda e: e.activation(out=osq[:], in_=o32[:], func=AF.Square), reads=["sb_o32"], writes=["sb_osq"])
                P.op("pe", lambda e: e.matmul(pG[:], ones, osq[:], start=True, stop=True), reads=["sb_osq", "cf"], writes=["pG"])
                P.op("act", lambda e: e.activation(out=lnt[:], in_=pG[:], func=AF.Ln, scale=1.0 / HD, bias=EPS), reads=["pG"], writes=["sb_lnt"])
                P.op("act", lambda e: e.activation(out=rst[:], in_=lnt[:], func=AF.Exp, scale=-0.5), reads=["sb_lnt"], writes=["sb_rst"])
                P.op("dve", lambda e, hh=hh, ob=ob, Q0=Q0: e.scalar_tensor_tensor(
                    out=ost[ob][:, Q0:Q0 + 512], in0=o32[:], scalar=gp_sb[:, hh:hh + 1], in1=rst[:], op0=ALU.mult, op1=ALU.mult),
                    reads=["sb_o32", "gp_sb", "sb_rst"], writes=[f"ost{ob}"])
            P.dma("sp", oT[out_idx], ost[ob][:], reads=[f"ost{ob}"], key=f"osto{ob}")
            out_idx += 1
    else:
        out_idx = 4

    if "swa" in parts:
        first_swa = True
        for hh in range(4):
            b = load_qkv(8 + hh, 12 if first_swa else None, 4 if first_swa else None)
            if first_swa:
                kw, vw, kkw, kvw = kb_[b], vb_[b], f"kb{b}", f"vb{b}"
                first_swa = False
            q = qb_[b]
            kq = f"qb{b}"
            ob = state["nost"] % 2
            state["nost"] += 1
            sw = scr("sw_s", [128, 256], F32)
            pws = [scr(f"sw_p{i}", [128, 256], BF16) for i in range(2)]
            nms = [scr(f"sw_nm{i}", [128, 1], F32) for i in range(2)]
            rss = [scr(f"sw_rs{i}", [128, 1], F32) for i in range(2)]
            es = scr("sw_es", [128, 1], F32)
            den = scr("sw_den", [128, 1], F32)
            rden = scr("rden", [128, 1], F32)
            ptw = scr("sw_pt", [128, 256], BF16)
            fronts, backs = [], []
            for i in range(nt_tok):
                q0 = i * 128
                qt = q[:, q0:q0 + 128]
                c_lo = 128 if i == 0 else 0
                k_lo = q0 - 128 + c_lo
                w = 256 - c_lo
                pb = i % 2
                pw, nm, rs = pws[pb], nms[pb], rss[pb]
                kpw, knm, krs = f"sw_p{pb}", f"sw_nm{pb}", f"sw_rs{pb}"
                _m0 = len(P.ops)
                P.op("pe", lambda e, pb=pb, qt=qt, k_lo=k_lo, w=w: e.matmul(pA[pb][:, 0:w], qt, kw[:, k_lo:k_lo + w], start=True, stop=True),
                     reads=[kq, kkw], writes=[f"pA{pb}"])
                P.op("dve", lambda e, pb=pb, hh=hh, c_lo=c_lo, w=w: e.tensor_tensor(out=sw[:, 0:w], in0=pA[pb][:, 0:w], in1=wb_sb[:, hh, c_lo:256], op=ALU.add),
                     reads=[f"pA{pb}", "wb_sb"], writes=["sw_s"])
                P.op("dve", lambda e, w=w, nm=nm: e.tensor_reduce(out=nm[:], in_=sw[:, 0:w], axis=AX.X, op=ALU.max),
                     reads=["sw_s"], writes=[knm])
                P.op("dve", lambda e, hh=hh, nm=nm: e.tensor_scalar(out=nm[:], in0=nm[:], scalar1=sink_sb[:, hh:hh + 1], scalar2=-1.0, op0=ALU.max, op1=ALU.mult),
                     reads=[knm, "sink_sb"], writes=[knm])
                P.op("act", lambda e, w=w, pw=pw, nm=nm, rs=rs: e.activation(out=pw[:, 0:w], in_=sw[:, 0:w], func=AF.Exp, bias=nm[:], accum_out=rs[:]),
                     reads=["sw_s", knm], writes=[kpw, krs])
                _m1 = len(P.ops)
                P.op("act", lambda e, hh=hh, nm=nm: e.activation(out=es[:], in_=sink_sb[:, hh:hh + 1], func=AF.Exp, bias=nm[:]),
                     reads=["sink_sb", knm], writes=["sw_es"])
                P.op("dve", lambda e, rs=rs: e.tensor_tensor(out=den[:], in0=rs[:], in1=es[:], op=ALU.add),
                     reads=[krs, "sw_es"], writes=["sw_den"])
                P.op("dve", lambda e: e.reciprocal(out=rden[:], in_=den[:]), reads=["sw_den"], writes=["rden"])
                nkt = w // 128
                for j in range(nkt):
                    P.op("pe", lambda e, j=j, pw=pw: e.transpose(pTs[0][:, j * 128:(j + 1) * 128], pw[:, j * 128:(j + 1) * 128], ident[:]),
                         reads=[kpw, "ident"], writes=["pTs0"])
                P.op("act", lambda e, w=w: e.activation(out=ptw[:, 0:w], in_=pTs[0][:, 0:w], func=AF.Copy),
                     reads=["pTs0"], writes=["sw_pt"])
                for j in range(nkt):
                    kt = (k_lo // 128) + j
                    P.op("pe", lambda e, j=j, kt=kt, nkt=nkt: e.matmul(pO[:, 0:128], ptw[:, j * 128:(j + 1) * 128], vw[:, kt, :], start=(j == 0), stop=(j == nkt - 1)),
                         reads=["sw_pt", kvw], writes=["pO"])
                head_norm_tok(pO[:, 0:128], "pO", rden[:], 2 + hh, ob, q0, "w")
                fronts.append(P.ops[_m0:_m1])
                backs.append(P.ops[_m1:])
                del P.ops[_m0:]
            P.ops.extend(fronts[0])
            for n_ in range(len(fronts)):
                if n_ + 1 < len(fronts):
                    P.ops.extend(fronts[n_ + 1])
                P.ops.extend(backs[n_])
            P.dma("sp", oT[out_idx], ost[ob][:], reads=[f"ost{ob}"], key=f"osto{ob}")
            out_idx += 1
    return P.emit()


D = 4096
KC = 32
TOK = 1024
TP = 512
EPS = 1e-6
NVEC = 7


def build_k3(NH, moe=False, npass=2, ngroups=None):
    NG = NH // 2
    if ngroups is None:
        ngroups = NG
    nc = bass.Bass("TRN2", target_bir_lowering=False)
    P = Prog(nc)
    dr = lambda n, sh, dt, kind="ExternalInput": nc.dram_tensor(n, list(sh), dt, kind=kind).ap()
    oT = dr("oT", [KC, 128, TOK], BF16)
    xT = dr("xT", [D, TOK], F32)
    vecs = dr("vecs", [128, NVEC * KC], F32)
    wout = dr("wout", [KC, 128, KC * 128], F32)
    wg = dr("wg", [NH, 128, KC * 128], F32)
    wu = dr("wu", [NH, 128, KC * 128], F32)
    wd = dr("wd", [NG, 2, 128, D], F32)
    if moe:
        wr = dr("wr", [128, KC * 8], F32)
        c_sel = dr("c_sel", [8, 8 * 128], F32)
        c_identf = dr("c_identf", [128, 128], F32)
    x1T = dr("x1T", [D, TOK], F32, kind="ExternalOutput")
    x2T = dr("x2T", [D, TOK], F32, kind="ExternalOutput")

    ones = P.sb([128, 128], F32, "ones")
    vec = P.sb([128, NVEC * KC], F32, "vec")
    gA = P.sb([128, KC], F32, "gA")
    gB = P.sb([128, KC], F32, "gB")
    gC = P.sb([128, KC], F32, "gC")
    hbuf = P.sb([128, KC, TP], BF16, "hbuf")
    yacc = P.sb([128, KC, TP], F32, "yacc")
    wgb = [P.sb([128, KC, 128], BF16, f"wgb{i}") for i in range(2)]
    wub = [P.sb([128, KC, 128], BF16, f"wub{i}") for i in range(2)]
    wdb = [P.sb([128, 2, D], BF16, f"wdb{i}") for i in range(2)]
    xc = [P.sb([128, TP], F32, f"xc{i}") for i in range(2)]
    sq = [P.sb([128, TP], F32, f"sq{i}") for i in range(2)]
    lnt = P.sb([128, TP], F32, "lnt")
    rstd = P.sb([128, TP], F32, "rstd")
    t1 = [P.sb([128, TP], F32, f"t1_{i}") for i in range(2)]
    sg = [P.sb([128, TP], F32, f"sg{i}") for i in range(2)]
    act = [P.sb([128, 2, TP], BF16, f"act{i}") for i in range(2)]
    pss = [P.ps([128, 512], F32, f"ps{i}") for i in range(8)]
    if moe:
        wr_sb = P.sb([128, KC, 8], F32, "wr_sb")
        sel = P.sb([8, 8 * 128], F32, "sel")
        identf = P.sb([128, 128], F32, "identf")
        lg = P.sb([128, 8], F32, "lg")
        mx8 = P.sb([128, 8], F32, "mx8")
        dd = P.sb([128, 1], F32, "dd")
        w1 = P.sb([128, 1], F32, "w1")
        w2 = P.sb([128, 1], F32, "w2")
        m1 = P.sb([128, 8], F32, "m1")
        comb = P.sb([128, 8], F32, "comb")
        combT = P.sb([8, TP], F32, "combT")
        combe = P.sb([128, TP], F32, "combe")
        P.dma("sp", wr_sb[:], wr.rearrange("p (kc e) -> p kc e", e=8), writes=["wr_sb"])
        P.dma("sp", sel[:], c_sel, writes=["sel"])
        P.dma("sp", identf[:], c_identf, writes=["identf"])

    P.op("pool", lambda e: e.memset(ones[:], 1.0), writes=["ones"])
    P.dma("sp", vec[:], vecs, writes=["vec"])
    V = lambda i: vec[:, i * KC:(i + 1) * KC]
    P.op("dve", lambda e: e.tensor_tensor(out=gA[:], in0=V(0), in1=V(1), op=ALU.mult), reads=["vec"], writes=["gA"])
    P.op("dve", lambda e: e.tensor_scalar(out=gB[:], in0=V(3), scalar1=1.0, scalar2=None, op0=ALU.add), reads=["vec"], writes=["gB"])
    P.op("dve", lambda e: e.tensor_tensor(out=gB[:], in0=gB[:], in1=V(2), op=ALU.mult), reads=["vec", "gB"], writes=["gB"])
    P.op("dve", lambda e: e.tensor_tensor(out=gC[:], in0=V(5), in1=V(6), op=ALU.mult), reads=["vec"], writes=["gC"])

    xT_v = xT.rearrange("(kc p) t -> p kc t", p=128)
    x1T_v = x1T.rearrange("(kc p) t -> p kc t", p=128)
    x2T_v = x2T.rearrange("(kc p) t -> p kc t", p=128)
    cnt = {"ps": 0, "w": 0, "ev": 0}

    def next_ps():
        cnt["ps"] += 1
        return 2 + (cnt["ps"] % 6)

    def rms_rstd(src_fn, key_fn):
        for kc in range(KC):
            b = kc % 2
            P.op("act", lambda e, kc=kc, b=b: e.activation(out=sq[b][:], in_=src_fn(kc), func=AF.Square),
                 reads=[key_fn(kc)], writes=[f"sq{b}"])
            P.op("pe", lambda e, kc=kc, b=b: e.matmul(pss[0][:], ones[:], sq[b][:], start=(kc == 0), stop=(kc == KC - 1)),
                 reads=["ones", f"sq{b}"], writes=["ps0"])
        P.op("act", lambda e: e.activation(out=lnt[:], in_=pss[0][:], func=AF.Ln, scale=1.0 / D, bias=EPS), reads=["ps0"], writes=["lnt"])
        P.op("act", lambda e: e.activation(out=rstd[:], in_=lnt[:], func=AF.Exp, scale=-0.5), reads=["lnt"], writes=["rstd"])

    for ps_ in range(npass):
        T0 = ps_ * TP
        P.dma("sp", hbuf[:], oT.rearrange("kc p t -> p kc t")[:, :, T0:T0 + TP], writes=["hbuf"])
        for c in range(KC):
            b = cnt["w"] % 2
            cnt["w"] += 1
            P.dma("pool", wgb[b][:], wout[c].rearrange("p (kc n) -> p kc n", n=128), writes=[f"wgb{b}"])
            pb = next_ps()
            for kc in range(KC):
                P.op("pe", lambda e, kc=kc, b=b, pb=pb: e.matmul(pss[pb][:], wgb[b][:, kc, :], hbuf[:, kc, :], start=(kc == 0), stop=(kc == KC - 1)),
                     reads=[f"wgb{b}", "hbuf"], writes=[f"ps{pb}"])
            if c % 2 == 0:
                P.op("act", lambda e, c=c, pb=pb: e.activation(out=yacc[:, c, :], in_=pss[pb][:], func=AF.Copy), reads=[f"ps{pb}"], writes=[f"y{c}"])
            else:
                P.op("dve", lambda e, c=c, pb=pb: e.tensor_copy(out=yacc[:, c, :], in_=pss[pb][:]), reads=[f"ps{pb}"], writes=[f"y{c}"])
        rms_rstd(lambda kc: yacc[:, kc, :], lambda kc: f"y{kc}")
        for kc in range(KC):
            b = kc % 2
            P.dma("sp", xc[b][:], xT_v[:, kc, T0:T0 + TP], writes=[f"xc{b}"])
            P.op("dve", lambda e, kc=kc, b=b: e.tensor_tensor(out=t1[b][:], in0=yacc[:, kc, :], in1=rstd[:], op=ALU.mult),
                 reads=[f"y{kc}", "rstd"], writes=[f"t1_{b}"])
            P.op("dve", lambda e, kc=kc, b=b: e.scalar_tensor_tensor(out=yacc[:, kc, :], in0=t1[b][:], scalar=gA[:, kc:kc + 1], in1=xc[b][:],
                                                                    op0=ALU.mult, op1=ALU.add),
                 reads=[f"t1_{b}", "gA", f"xc{b}"], writes=[f"y{kc}"])
            P.dma("sp", x1T_v[:, kc, T0:T0 + TP], yacc[:, kc, :], reads=[f"y{kc}"], writes=["x1dram"], key="x1dram")
        rms_rstd(lambda kc: yacc[:, kc, :], lambda kc: f"y{kc}")
        if moe:
            pass
        for kc in range(KC):
            b = kc % 2
            P.op("dve", lambda e, kc=kc, b=b: e.scalar_tensor_tensor(out=t1[b][:], in0=yacc[:, kc, :], scalar=gB[:, kc:kc + 1], in1=rstd[:],
                                                                    op0=ALU.mult, op1=ALU.mult),
                 reads=[f"y{kc}", "gB", "rstd"], writes=[f"t1_{b}"])
            if moe:
                P.op("act", lambda e, kc=kc, b=b: e.activation(out=sq[b][:], in_=t1[b][:], func=AF.Identity, bias=vec[:, 4 * KC + kc:4 * KC + kc + 1]),
                     reads=[f"t1_{b}", "vec"], writes=[f"sq{b}"])
                P.op("pool", lambda e, kc=kc, b=b: e.tensor_copy(out=hbuf[:, kc, :], in_=sq[b][:]), reads=[f"sq{b}"], writes=["hbuf"])
                for tt in range(4):
                    P.op("pe", lambda e, kc=kc, b=b, tt=tt: e.matmul(pss[1 + tt][:, 0:8], sq[b][:, tt * 128:(tt + 1) * 128], wr_sb[:, kc, :],
                                                                     start=(kc == 0), stop=(kc == KC - 1)),
                         reads=[f"sq{b}", "wr_sb"], writes=[f"ps{1 + tt}"])
            else:
                P.op("act", lambda e, kc=kc, b=b: e.activation(out=hbuf[:, kc, :], in_=t1[b][:], func=AF.Identity, bias=vec[:, 4 * KC + kc:4 * KC + kc + 1]),
                     reads=[f"t1_{b}", "vec"], writes=["hbuf"])
        if moe:
            for tt in range(4):
                P.op("dve", lambda e, tt=tt: e.tensor_copy(out=lg[:], in_=pss[1 + tt][:, 0:8]), reads=[f"ps{1 + tt}"], writes=["lg"])
                P.op("dve", lambda e: e.max(out=mx8[:], in_=lg[:]), reads=["lg"], writes=["mx8"])
                P.op("dve", lambda e: e.tensor_tensor(out=dd[:], in0=mx8[:, 1:2], in1=mx8[:, 0:1], op=ALU.subtract), reads=["mx8"], writes=["dd"])
                P.op("act", lambda e: e.activation(out=w2[:], in_=dd[:], func=AF.Exp), reads=["dd"], writes=["w2"])
                P.op("dve", lambda e: e.tensor_scalar(out=w1[:], in0=w2[:], scalar1=1.0, scalar2=None, op0=ALU.add), reads=["w2"], writes=["w1"])
                P.op("dve", lambda e: e.reciprocal(out=w1[:], in_=w1[:]), reads=["w1"], writes=["w1"])
                P.op("dve", lambda e: e.tensor_tensor(out=w2[:], in0=w2[:], in1=w1[:], op=ALU.mult), reads=["w1", "w2"], writes=["w2"])
                P.op("dve", lambda e: e.tensor_scalar(out=m1[:], in0=lg[:], scalar1=mx8[:, 0:1], scalar2=w1[:], op0=ALU.is_equal, op1=ALU.mult),
                     reads=["lg", "mx8", "w1"], writes=["m1"])
                P.op("dve", lambda e: e.tensor_scalar(out=comb[:], in0=lg[:], scalar1=mx8[:, 1:2], scalar2=w2[:], op0=ALU.is_equal, op1=ALU.mult),
                     reads=["lg", "mx8", "w2"], writes=["comb"])
                P.op("dve", lambda e: e.tensor_tensor(out=comb[:], in0=comb[:], in1=m1[:], op=ALU.add), reads=["comb", "m1"], writes=["comb"])
                P.op("pe", lambda e: e.transpose(pss[0][0:8, 0:128], comb[:], identf[:]), reads=["comb", "identf"], writes=["ps0"])
                P.op("act", lambda e, tt=tt: e.activation(out=combT[:, tt * 128:(tt + 1) * 128], in_=pss[0][0:8, 0:128], func=AF.Copy),
                     reads=["ps0"], writes=["combT"])
        for gi in range(ngroups):
            b = gi % 2
            P.dma("pool", wdb[b][:], wd[gi].rearrange("j p n -> p j n"), writes=[f"wdb{b}"])
            if moe and gi % 16 == 0:
                ex = gi // 16
                P.op("pe", lambda e, ex=ex: e.matmul(pss[1][:], sel[:, ex * 128:(ex + 1) * 128], combT[:], start=True, stop=True),
                     reads=["sel", "combT"], writes=["ps1"])
                P.op("act", lambda e: e.activation(out=combe[:], in_=pss[1][:], func=AF.Copy), reads=["ps1"], writes=["combe"])
            ab = gi % 2
            for jj in range(2):
                wb_ = cnt["w"] % 2
                cnt["w"] += 1
                P.dma("pool", wgb[wb_][:], wg[2 * gi + jj].rearrange("p (kc n) -> p kc n", n=128), writes=[f"wgb{wb_}"])
                P.dma("pool", wub[wb_][:], wu[2 * gi + jj].rearrange("p (kc n) -> p kc n", n=128), writes=[f"wub{wb_}"])
                pg = next_ps()
                for kc in range(KC):
                    P.op("pe", lambda e, kc=kc, wb_=wb_, pg=pg: e.matmul(pss[pg][:], wgb[wb_][:, kc, :], hbuf[:, kc, :],
                                                                         start=(kc == 0), stop=(kc == KC - 1)),
                         reads=[f"wgb{wb_}", "hbuf"], writes=[f"ps{pg}"])
                pu = next_ps()
                for kc in range(KC):
                    P.op("pe", lambda e, kc=kc, wb_=wb_, pu=pu: e.matmul(pss[pu][:], wub[wb_][:, kc, :], hbuf[:, kc, :],
                                                                         start=(kc == 0), stop=(kc == KC - 1)),
                         reads=[f"wub{wb_}", "hbuf"], writes=[f"ps{pu}"])
                P.op("act", lambda e, jj=jj, pg=pg: e.activation(out=sg[jj][:], in_=pss[pg][:], func=AF.Silu), reads=[f"ps{pg}"], writes=[f"sg{jj}"])
                if moe:
                    P.op("dve", lambda e, jj=jj, pu=pu: e.tensor_tensor(out=sg[jj][:], in0=sg[jj][:], in1=pss[pu][:], op=ALU.mult),
                         reads=[f"sg{jj}", f"ps{pu}"], writes=[f"sg{jj}"])
                    P.op("dve", lambda e, jj=jj, ab=ab: e.tensor_tensor(out=act[ab][:, jj, :], in0=sg[jj][:], in1=combe[:], op=ALU.mult),
                         reads=[f"sg{jj}", "combe"], writes=[f"act{ab}"])
                else:
                    P.op("dve", lambda e, jj=jj, pu=pu, ab=ab: e.tensor_tensor(out=act[ab][:, jj, :], in0=sg[jj][:], in1=pss[pu][:], op=ALU.mult),
                         reads=[f"sg{jj}", f"ps{pu}"], writes=[f"act{ab}"])
            for c in range(KC):
                pd = next_ps()
                for jj in range(2):
                    P.op("pe", lambda e, c=c, jj=jj, pd=pd, b=b, ab=ab: e.matmul(pss[pd][:], wdb[b][:, jj, c * 128:(c + 1) * 128], act[ab][:, jj, :],
                                                                                 start=(jj == 0), stop=(jj == 1)),
                         reads=[f"wdb{b}", f"act{ab}"], writes=[f"ps{pd}"])
                if gi == 0:
                    P.op("dve", lambda e, c=c, pd=pd: e.tensor_copy(out=yacc[:, c, :], in_=pss[pd][:]), reads=[f"ps{pd}"], writes=[f"y{c}"])
                else:
                    P.op("dve", lambda e, c=c, pd=pd: e.tensor_tensor(out=yacc[:, c, :], in0=yacc[:, c, :], in1=pss[pd][:], op=ALU.add),
                         reads=[f"ps{pd}", f"y{c}"], writes=[f"y{c}"])
        rms_rstd(lambda kc: yacc[:, kc, :], lambda kc: f"y{kc}")
        for kc in range(KC):
            b = kc % 2
            P.dma("sp", xc[b][:], x1T_v[:, kc, T0:T0 + TP], reads=["x1dram"], writes=[f"xc{b}"])
            P.op("dve", lambda e, kc=kc, b=b: e.tensor_tensor(out=t1[b][:], in0=yacc[:, kc, :], in1=rstd[:], op=ALU.mult),
                 reads=[f"y{kc}", "rstd"], writes=[f"t1_{b}"])
            P.op("dve", lambda e, kc=kc, b=b: e.scalar_tensor_tensor(out=t1[b][:], in0=t1[b][:], scalar=gC[:, kc:kc + 1], in1=xc[b][:],
                                                                    op0=ALU.mult, op1=ALU.add),
                 reads=[f"t1_{b}", "gC", f"xc{b}"], writes=[f"t1_{b}"])
            P.dma("sp", x2T_v[:, kc, T0:T0 + TP], t1[b][:], reads=[f"t1_{b}"], key=f"xoo{b}")
    return P.emit()


def tile_cols(w, width):
    K, N = w.shape
    return np.ascontiguousarray(w.reshape(K // 128, 128, N // width, width).transpose(2, 1, 0, 3).reshape(N // width, 128, (K // 128) * width))


from concourse.bass_utils import run_bass_kernel_spmd

N_CORES = 8
_PROGS = {}


def _prog(name, fn):
    if name not in _PROGS:
        _PROGS[name] = fn()
    return _PROGS[name]


def _lay(v):
    return np.asarray(v, np.float32).reshape(KC, 128).T


def _run(nc, in_maps):
    res = run_bass_kernel_spmd(nc, in_maps, core_ids=list(range(N_CORES)))
    return res.results


def kernel(x, c, rel_bias, w_ada, b_ada, g_pre_mix, w_in, g_grp_moba, g_grp_sb, g_grp_swa, swa_sinks, w_out,
           g_post_mix, g_pre_ffn, w_ff_gate, w_ff_up, w_ff_down, w_router, w_moe_gate, w_moe_up, w_moe_down, g_post_ffn):
    f32 = np.float32
    x = np.asarray(x, f32)
    c = np.asarray(c, f32)
    rel_bias = np.asarray(rel_bias, f32)
    w_ada = np.asarray(w_ada, f32)
    b_ada = np.asarray(b_ada, f32)
    B, S_, D_ = x.shape
    oh = build_onehot()
    cT = np.ascontiguousarray(c.T.reshape(KC, 128, 2).transpose(1, 0, 2).reshape(128, KC * 2))
    rbaug = np.concatenate([rel_bias, np.full((1, 24), NEG, f32)], axis=0)
    ims = []
    for core in range(N_CORES):
        sl = slice(core * ADA_CORE, (core + 1) * ADA_CORE)
        wt = w_ada[:, :, sl].reshape(2, KC, 128, 6, 512).transpose(0, 3, 2, 1, 4).reshape(12, 128, KC * 512)
        ims.append({"cT": cT, "wada": np.ascontiguousarray(wt),
                    "bada": np.ascontiguousarray(np.broadcast_to(b_ada[:, sl].reshape(1, 2 * ADA_CORE), (2, 2 * ADA_CORE))),
                    "rbaug": rbaug, "oh": np.ascontiguousarray(oh[:, core * N_TAB_CORE:(core + 1) * N_TAB_CORE])})
    r0 = _run(_prog("k0", build_k0), ims)
    del ims
    mod = np.concatenate([np.asarray(r["modo"]) for r in r0], axis=2)
    tab = np.concatenate([np.asarray(r["tabo"]) for r in r0], axis=1)
    wtab = tab[:, 0:128 * 256].reshape(24, 128, 256)
    mtab = tab[:, 128 * 256:].reshape(24, 2, 128, 512)
    consts2 = {}
    consts2["c_ident"] = np.eye(128, dtype=f32).astype(ml_dtypes.bfloat16)
    jp = np.arange(128)[:, None]
    jj = np.arange(128)[None, :]
    consts2["c_f32"] = np.stack([-(jp >= jj).astype(f32), -np.ones((128, 128), f32), np.ones((128, 128), f32)])
    ff = np.arange(512)[None, :]
    consts2["c_sbmask"] = np.ascontiguousarray(np.concatenate([(ff > 128 * o + jp).astype(f32) for o in range(4)], axis=1))
    selc = np.zeros((8, 8 * 128), f32)
    for e_ in range(8):
        selc[e_, e_ * 128:(e_ + 1) * 128] = 1

    xcur = x
    for l in range(2):
        sh_m, sc_m, gt_m, sh_f, sc_f, gt_f = [mod[l][:, i * D:(i + 1) * D] for i in range(6)]
        wfm, wv = k1_host_weights(np.asarray(w_in[l], f32))
        ims = []
        for core in range(N_CORES):
            b, ts = core // 4, core % 4
            xT = np.ascontiguousarray(xcur[b, ts * TOK:(ts + 1) * TOK, :].T)
            vecs = np.ascontiguousarray(np.concatenate([_lay(g_pre_mix[l]), _lay(sc_m[b]), _lay(sh_m[b])], axis=1))
            ims.append({"xT": xT, "vecs": vecs, "wfm": wfm, "wv": wv})
        r1 = _run(_prog("k1", build_k1), ims)
        del ims, wfm, wv
        QK = [np.concatenate([np.asarray(r1[b * 4 + ts]["outT"]) for ts in range(4)], axis=2) for b in range(B)]
        VV = [np.concatenate([np.asarray(r1[b * 4 + ts]["outV"]) for ts in range(4)], axis=0) for b in range(B)]
        del r1
        ims = []
        for core in range(N_CORES):
            b, g = core // 4, core % 4
            qi = []
            for hh in range(2):
                qi += [2 * g + hh, 8 + 2 * g + hh]
            for hh in range(2):
                qi += [16 + 2 * g + hh, 24 + 2 * g + hh]
            qi += [32 + 4 * g + i for i in range(4)]
            qi += [48 + g // 2]
            qk = np.ascontiguousarray(QK[b][qi])
            vcols = [(2 * g) * 128, (2 * g + 1) * 128, 1024 + (2 * g) * 128, 1024 + (2 * g + 1) * 128, 2048 + (g // 2) * 128]
            vv = np.ascontiguousarray(np.stack([VV[b][:, c0:c0 + 128] for c0 in vcols]))
            mh = [2 * g, 2 * g + 1]
            wh = [4 * g + i for i in range(4)]
            gq = np.stack([np.broadcast_to(np.asarray(g_grp_moba[l], f32)[h * 128:(h + 1) * 128][None], (128, 128)) for h in mh] +
                          [np.broadcast_to(np.asarray(g_grp_swa[l], f32)[h * 128:(h + 1) * 128][None], (128, 128)) for h in wh])
            gp = np.ascontiguousarray(np.stack([np.asarray(g_grp_sb[l], f32)[h * 128:(h + 1) * 128] for h in mh], axis=1))
            im = {"qk": qk, "vv": vv,
                  "mbias": np.ascontiguousarray(mtab[mh]),
                  "mtab31": np.ascontiguousarray(np.broadcast_to(rel_bias[31, mh][None, :], (128, 2))),
                  "wbias": np.ascontiguousarray(wtab[[8 + h for h in wh]]),
                  "sinks": np.ascontiguousarray(np.broadcast_to(np.asarray(swa_sinks[l], f32)[wh][None, :], (128, 4))),
                  "gq": np.ascontiguousarray(gq.astype(f32)), "gp": gp,
                  "c_ident": consts2["c_ident"], "c_f32": consts2["c_f32"], "c_sbmask": consts2["c_sbmask"]}
            ims.append(im)
        r2 = _run(_prog("k2", build_k2), ims)
        del ims, QK, VV
        OT = []
        for b in range(B):
            ch = []
            for h in range(8):
                ch.append(np.asarray(r2[b * 4 + h // 2]["oT"])[h % 2])
            for h in range(8):
                ch.append(np.asarray(r2[b * 4 + h // 2]["oT"])[2 + h % 2])
            for h in range(16):
                ch.append(np.asarray(r2[b * 4 + h // 4]["oT"])[4 + h % 4])
            OT.append(np.stack(ch))
        del r2
        wout_t = tile_cols(np.asarray(w_out[l], f32), 128)
        i = l // 2
        if l % 2 == 0:
            wg_t = tile_cols(np.asarray(w_ff_gate[i], f32), 128)
            wu_t = tile_cols(np.asarray(w_ff_up[i], f32), 128)
            wd_t = np.ascontiguousarray(np.asarray(w_ff_down[i], f32).reshape(-1, 2, 128, D))
            prog = _prog("k3d", lambda: build_k3(112, moe=False))
            extra = {}
        else:
            wg_t = np.concatenate([tile_cols(np.asarray(w_moe_gate[i, e_], f32), 128) for e_ in range(8)], axis=0)
            wu_t = np.concatenate([tile_cols(np.asarray(w_moe_up[i, e_], f32), 128) for e_ in range(8)], axis=0)
            wd_t = np.ascontiguousarray(np.asarray(w_moe_down[i], f32).reshape(-1, 2, 128, D))
            prog = _prog("k3m", lambda: build_k3(256, moe=True))
            extra = {"wr": np.ascontiguousarray(np.asarray(w_router[i], f32).reshape(KC, 128, 8).transpose(1, 0, 2).reshape(128, KC * 8)),
                     "c_sel": selc, "c_identf": np.eye(128, dtype=f32)}
        ims = []
        for core in range(N_CORES):
            b, ts = core // 4, core % 4
            xT = np.ascontiguousarray(xcur[b, ts * TOK:(ts + 1) * TOK, :].T)
            vecs = np.ascontiguousarray(np.concatenate(
                [_lay(g_post_mix[l]), _lay(gt_m[b]), _lay(g_pre_ffn[l]), _lay(sc_f[b]), _lay(sh_f[b]), _lay(g_post_ffn[l]), _lay(gt_f[b])], axis=1))
            im = {"oT": np.ascontiguousarray(OT[b][:, :, ts * TOK:(ts + 1) * TOK]), "xT": xT, "vecs": vecs,
                  "wout": wout_t, "wg": wg_t, "wu": wu_t, "wd": wd_t}
            im.update(extra)
            ims.append(im)
        r3 = _run(prog, ims)
        del ims, wg_t, wu_t, wd_t, wout_t, OT
        xn = np.empty((B, S_, D_), f32)
        for core in range(N_CORES):
            b, ts = core // 4, core % 4
            xn[b, ts * TOK:(ts + 1) * TOK, :] = np.asarray(r3[core]["x2T"]).T
        del r3
        xcur = xn
    return xcur
```

```python
import ml_dtypes
import numpy as np
from contextlib import ExitStack
import concourse.bass as bass
import concourse.mybir as mybir

F32 = mybir.dt.float32
BF16 = mybir.dt.bfloat16
AF = mybir.ActivationFunctionType
ALU = mybir.AluOpType
AX = mybir.AxisListType

SAME_ENGINE_SYNC = True


class Op:
    __slots__ = ("eng", "fn", "reads", "writes", "dma_key", "signal", "cnt", "waits")

    def __init__(self, eng, fn, reads, writes, dma_key):
        self.eng = eng
        self.fn = fn
        self.reads = reads
        self.writes = writes
        self.dma_key = dma_key
        self.signal = False
        self.cnt = 0
        self.waits = None


class Prog:
    ENGS = ("pe", "act", "dve", "pool", "sp")

    def __init__(self, nc):
        self.nc = nc
        self.ops = []
        self.ctx = ExitStack()
        self._n = 0
        self.excl = set()

    def sb(self, shape, dt, name=None):
        self._n += 1
        return self.ctx.enter_context(self.nc.sbuf_tensor(name or f"sb{self._n}", list(shape), dt))

    def ps(self, shape, dt, name=None):
        self._n += 1
        if name:
            self.excl.add(name)
        return self.ctx.enter_context(self.nc.psum_tensor(name or f"ps{self._n}", list(shape), dt))

    def op(self, eng, fn, reads=(), writes=()):
        self.ops.append(Op(eng, fn, tuple(reads), tuple(writes), None))

    def dma(self, eng, out, in_, reads=(), writes=(), key=None):
        if key is None:
            key = (tuple(writes) + tuple(reads))[0]
        self.ops.append(Op(eng, lambda e: e.dma_start(out=out, in_=in_), tuple(reads), tuple(writes), "dma:" + str(key)))

    def emit(self):
        nc = self.nc
        ops = self.ops
        last_writer = {}
        readers = {}
        deps = [None] * len(ops)
        for i, op in enumerate(ops):
            d = set()
            for k in op.reads:
                if k in last_writer:
                    d.add(last_writer[k])
                if k in self.excl:
                    for r in readers.get(k, ()):
                        if ops[r].eng != op.eng:
                            d.add(r)
            for k in op.writes:
                if k in last_writer:
                    d.add(last_writer[k])
                for r in readers.get(k, ()):
                    d.add(r)
            for k in op.reads:
                readers.setdefault(k, []).append(i)
            for k in op.writes:
                last_writer[k] = i
                readers[k] = []
            d.discard(i)
            deps[i] = d
        for i, op in enumerate(ops):
            for j in deps[i]:
                pj = ops[j]
                if pj.dma_key is not None:
                    pj.signal = True
                elif pj.eng != op.eng or (SAME_ENGINE_SYNC and op.eng != "pe"):
                    pj.signal = True
        eng_cnt = {e: 0 for e in self.ENGS}
        dma_cnt = {}
        for op in ops:
            if op.dma_key is not None:
                dma_cnt[op.dma_key] = dma_cnt.get(op.dma_key, 0) + 16
                op.cnt = dma_cnt[op.dma_key]
                op.signal = True
            elif op.signal:
                eng_cnt[op.eng] += 1
                op.cnt = eng_cnt[op.eng]
        sems = {}
        for e in self.ENGS:
            sems["eng:" + e] = self.ctx.enter_context(nc.semaphore("s_" + e))
        for n, k in enumerate(sorted(dma_cnt)):
            sems[k] = self.ctx.enter_context(nc.semaphore(f"s_dma{n}"))
        self.n_sems = len(sems)
        waited = {e: {} for e in self.ENGS}
        for i, op in enumerate(ops):
            need = {}
            for j in deps[i]:
                pj = ops[j]
                if pj.dma_key is not None:
                    sk = pj.dma_key
                elif pj.eng != op.eng or (SAME_ENGINE_SYNC and op.eng != "pe"):
                    sk = "eng:" + pj.eng
                else:
                    continue
                if pj.cnt > need.get(sk, 0):
                    need[sk] = pj.cnt
            w = []
            for sk, v in need.items():
                if waited[op.eng].get(sk, 0) < v:
                    waited[op.eng][sk] = v
                    w.append((sk, v))
            op.waits = w
        final_waits = [(k, v) for k, v in dma_cnt.items()]
        per_eng = {e: [op for op in ops if op.eng == e] for e in self.ENGS}
        block = self.ctx.enter_context(nc.Block())

        def run(e_name, eng):
            for op in per_eng[e_name]:
                for sk, v in op.waits:
                    eng.wait_ge(sems[sk], v)
                ins = op.fn(eng)
                if op.signal:
                    if op.dma_key is not None:
                        ins.then_inc(sems[op.dma_key], 16)
                    else:
                        ins.then_inc(sems["eng:" + e_name], 1)
            if e_name == "sp":
                for sk, v in final_waits:
                    eng.wait_ge(sems[sk], v)

        @block.tensor
        def _(e):
            run("pe", e)

        @block.scalar
        def _(e):
            run("act", e)

        @block.vector
        def _(e):
            run("dve", e)

        @block.gpsimd
        def _(e):
            run("pool", e)

        @block.sync
        def _(e):
            run("sp", e)

        self.ctx.close()
        return nc


D = 4096
KC = 32
NEG = -30000.0
N_TAB = 128 * 256 + 2 * 128 * 512
N_TAB_CORE = N_TAB // 8
ADA_CORE = 24576 // 8


def rel_bucket_np(dist):
    import math
    n = np.maximum(dist, 0)
    nf = np.maximum(n, 1).astype(np.float32)
    v = (np.log(nf / np.float32(16)) / np.float32(math.log(128 / 16)) * np.float32(16)).astype(np.float32)
    large = 16 + v.astype(np.int32)
    return np.where(n < 16, n, np.minimum(large, 31)).astype(np.int64)


def build_onehot():
    p = np.arange(128)[:, None]
    cols = []
    j = np.arange(256)[None, :]
    dist = p + 128 - j
    valid = (dist >= 0) & (dist < 128)
    cols.append((np.where(valid, rel_bucket_np(dist), 32)).reshape(-1))
    for qpos in range(2):
        dprev = 256 + qpos * 128 + p - j
        down = qpos * 128 + p - j
        blk = np.concatenate([rel_bucket_np(dprev), np.where(down >= 0, rel_bucket_np(down), 32)], axis=1)
        cols.append(blk.reshape(-1))
    idx = np.concatenate(cols)
    oh = np.zeros((33, N_TAB), np.float32)
    oh[idx, np.arange(N_TAB)] = 1.0
    return oh


def build_k0():
    nc = bass.Bass("TRN2", target_bir_lowering=False)
    P = Prog(nc)
    dr = lambda n, sh, dt, kind="ExternalInput": nc.dram_tensor(n, list(sh), dt, kind=kind).ap()
    cT = dr("cT", [128, KC * 2], F32)
    wada = dr("wada", [12, 128, KC * 512], F32)
    bada = dr("bada", [2, 2 * ADA_CORE], F32)
    rbaug = dr("rbaug", [33, 24], F32)
    oh = dr("oh", [33, N_TAB_CORE], F32)
    modo = dr("modo", [2, 2, ADA_CORE], F32, kind="ExternalOutput")
    tabo = dr("tabo", [24, N_TAB_CORE], F32, kind="ExternalOutput")

    cs = P.sb([128, KC, 2], F32, "cs")
    wb = [P.sb([128, KC, 512], F32, f"wb{i}") for i in range(2)]
    bsb = P.sb([2, 2 * ADA_CORE], F32, "bsb")
    rb = P.sb([33, 24], F32, "rb")
    ohs = [P.sb([33, 2048], F32, f"ohs{i}") for i in range(2)]
    mo = [P.sb([2, 512], F32, f"mo{i}") for i in range(2)]
    to = [P.sb([24, 512], F32, f"to{i}") for i in range(2)]
    pss = [P.ps([128, 512], F32, f"ps{i}") for i in range(4)]

    P.dma("sp", cs[:], cT.rearrange("p (kc b) -> p kc b", b=2), writes=["cs"])
    P.dma("sp", bsb[:], bada, writes=["bsb"])
    P.dma("sp", rb[:], rbaug, writes=["rb"])
    P.op("act", lambda e: e.activation(out=cs[:], in_=cs[:], func=AF.Silu), reads=["cs"], writes=["cs"])
    n = 0
    for l in range(2):
        for g in range(6):
            b = n % 2
            P.dma("sp", wb[b][:], wada[l * 6 + g].rearrange("p (kc n) -> p kc n", n=512), writes=[f"wb{b}"])
            for kc in range(KC):
                P.op("pe", lambda e, kc=kc, b=b: e.matmul(pss[b][0:2, :], cs[:, kc, :], wb[b][:, kc, :], start=(kc == 0), stop=(kc == KC - 1)),
                     reads=["cs", f"wb{b}"], writes=[f"ps{b}"])
            P.op("dve", lambda e, b=b, l=l, g=g: e.tensor_tensor(out=mo[b][:], in0=pss[b][0:2, :],
                                                                in1=bsb[:, l * ADA_CORE + g * 512:l * ADA_CORE + (g + 1) * 512], op=ALU.add),
                 reads=[f"ps{b}", "bsb"], writes=[f"mo{b}"])
            P.dma("sp", modo[l, :, g * 512:(g + 1) * 512], mo[b][:], reads=[f"mo{b}"], key=f"moo{b}")
            n += 1
    for t in range(N_TAB_CORE // 512):
        b = t % 2
        ob = (t // 4) % 2
        if t % 4 == 0:
            P.dma("sp", ohs[ob][:], oh[:, t * 512:t * 512 + 2048], writes=[f"ohs{ob}"])
        P.op("pe", lambda e, t=t, b=b, ob=ob: e.matmul(pss[2 + b][0:24, :], rb[:], ohs[ob][:, (t % 4) * 512:(t % 4 + 1) * 512], start=True, stop=True),
             reads=["rb", f"ohs{ob}"], writes=[f"ps{2 + b}"])
        P.op("act", lambda e, b=b: e.activation(out=to[b][:], in_=pss[2 + b][0:24, :], func=AF.Copy), reads=[f"ps{2 + b}"], writes=[f"to{b}"])
        P.dma("sp", tabo[:, t * 512:(t + 1) * 512], to[b][:], reads=[f"to{b}"], key=f"too{b}")
    return P.emit()


D = 4096
KC = 32
TOK = 1024
IN_W = 8704
FM_CHUNKS = list(range(0, 16)) + list(range(24, 40)) + list(range(48, 66))
Q_CHUNKS = set(list(range(0, 8)) + list(range(24, 32)) + list(range(48, 64)))
V_COLS = list(range(16 * 128, 24 * 128)) + list(range(40 * 128, 48 * 128)) + list(range(66 * 128, 68 * 128))
V_GROUPS = [(0, 512), (512, 512), (1024, 512), (1536, 512), (2048, 256)]
NV = 2304
QSCALE = 128 ** -0.5
EPS = 1e-6


def build_k1():
    nc = bass.Bass("TRN2", target_bir_lowering=False)
    P = Prog(nc)
    xT = nc.dram_tensor("xT", [D, TOK], F32, kind="ExternalInput").ap()
    vecs = nc.dram_tensor("vecs", [128, 3 * KC], F32, kind="ExternalInput").ap()
    wfm = nc.dram_tensor("wfm", [50, 128, KC * 128], F32, kind="ExternalInput").ap()
    wv = nc.dram_tensor("wv", [128, KC * NV], F32, kind="ExternalInput").ap()
    outT = nc.dram_tensor("outT", [50, 128, TOK], BF16, kind="ExternalOutput").ap()
    outV = nc.dram_tensor("outV", [TOK, NV], BF16, kind="ExternalOutput").ap()

    ones = P.sb([128, 128], F32, "ones")
    vec = P.sb([128, 3 * KC], F32, "vec")
    gs = P.sb([128, KC], F32, "gs")
    h = P.sb([128, KC, TOK], BF16, "h")
    NT = 128
    NTT = TOK // NT
    xb = [P.sb([128, KC, NT], F32, f"xb{i}") for i in range(2)]
    sq = P.sb([128, KC, NT], F32, "sq")
    lnt = P.sb([128, NT], F32, "lnt")
    rstd = P.sb([128, NT], F32, "rstd")
    t1 = [P.sb([128, NT], F32, f"t1_{i}") for i in range(2)]
    wb = [P.sb([128, KC, 128], BF16, f"wb{i}") for i in range(2)]
    wvb = [P.sb([128, KC, 512], BF16, f"wvb{i}") for i in range(2)]
    stg = [P.sb([128, TOK], BF16, f"stg{i}") for i in range(2)]
    stv = [P.sb([128, 512], BF16, f"stv{i}") for i in range(2)]
    pss = [P.ps([128, 512], F32, f"psb{i}") for i in range(8)]

    P.op("pool", lambda e: e.memset(ones[:], 1.0), writes=["ones"])
    P.dma("sp", vec[:], vecs, writes=["vec"])
    P.op("dve", lambda e: e.tensor_scalar(out=gs[:], in0=vec[:, KC:2 * KC], scalar1=1.0, scalar2=None, op0=ALU.add),
         reads=["vec"], writes=["gs"])
    P.op("dve", lambda e: e.tensor_tensor(out=gs[:], in0=gs[:], in1=vec[:, 0:KC], op=ALU.mult),
         reads=["vec", "gs"], writes=["gs"])

    xT_v = xT.rearrange("(kc p) t -> p kc t", p=128)
    for tt in range(NTT):
        b = tt % 2
        P.dma("sp", xb[b][:], xT_v[:, :, tt * NT:(tt + 1) * NT], writes=[f"xb{b}"])
        P.op("act", lambda e, b=b: e.activation(out=sq[:], in_=xb[b][:], func=AF.Square),
             reads=[f"xb{b}"], writes=["sq"])
        for kc in range(KC):
            P.op("pe", lambda e, kc=kc: e.matmul(pss[0][:, 0:NT], ones[:], sq[:, kc, :], start=(kc == 0), stop=(kc == KC - 1)),
                 reads=["ones", "sq"], writes=["ps0"])
        P.op("act", lambda e: e.activation(out=lnt[:], in_=pss[0][:, 0:NT], func=AF.Ln, scale=1.0 / D, bias=EPS),
             reads=["ps0"], writes=["lnt"])
        P.op("act", lambda e: e.activation(out=rstd[:], in_=lnt[:], func=AF.Exp, scale=-0.5),
             reads=["lnt"], writes=["rstd"])
        for kc in range(KC):
            tb = kc % 2
            P.op("dve", lambda e, kc=kc, tb=tb, b=b: e.scalar_tensor_tensor(
                out=t1[tb][:], in0=xb[b][:, kc, :], scalar=gs[:, kc:kc + 1], in1=rstd[:], op0=ALU.mult, op1=ALU.mult),
                reads=[f"xb{b}", "gs", "rstd"], writes=[f"t1_{tb}"])
            P.op("act", lambda e, kc=kc, tb=tb, tt=tt: e.activation(
                out=h[:, kc, tt * NT:(tt + 1) * NT], in_=t1[tb][:], func=AF.Identity, bias=vec[:, 2 * KC + kc:2 * KC + kc + 1]),
                reads=[f"t1_{tb}", "vec"], writes=[f"h{tt}"])
    hkeys = [f"h{tt}" for tt in range(NTT)]

    n_ps = 0
    for ci in range(50):
        c = FM_CHUNKS[ci]
        b = ci % 2
        P.dma("pool", wb[b][:], wfm[ci].rearrange("p (kc n) -> p kc n", n=128), writes=[f"wb{b}"])
        for t2 in range(2):
            pb = 2 + (n_ps % 4)
            n_ps += 1
            for kc in range(KC):
                P.op("pe", lambda e, kc=kc, b=b, t2=t2, pb=pb: e.matmul(
                    pss[pb][:], wb[b][:, kc, :], h[:, kc, t2 * 512:(t2 + 1) * 512], start=(kc == 0), stop=(kc == KC - 1)),
                    reads=[f"wb{b}"] + hkeys[4 * t2:4 * t2 + 4], writes=[f"ps{pb}"])
            sc = QSCALE if c in Q_CHUNKS else 1.0
            if n_ps % 2 == 0:
                P.op("act", lambda e, b=b, t2=t2, pb=pb, sc=sc: e.activation(
                    out=stg[b][:, t2 * 512:(t2 + 1) * 512], in_=pss[pb][:], func=AF.Copy, scale=sc),
                    reads=[f"ps{pb}"], writes=[f"stg{b}"])
            else:
                P.op("dve", lambda e, b=b, t2=t2, pb=pb, sc=sc: e.tensor_scalar(
                    out=stg[b][:, t2 * 512:(t2 + 1) * 512], in0=pss[pb][:], scalar1=sc, scalar2=None, op0=ALU.mult),
                    reads=[f"ps{pb}"], writes=[f"stg{b}"])
        P.dma("sp", outT[ci], stg[b][:], reads=[f"stg{b}"], key=f"stgo{b}")

    wv_off = 0
    nst = 0
    for gi, (c0, wdt) in enumerate(V_GROUPS):
        b = gi % 2
        P.dma("pool", wvb[b][:, :, 0:wdt], wv[:, wv_off:wv_off + KC * wdt].rearrange("p (kc n) -> p kc n", n=wdt),
              writes=[f"wvb{b}"])
        wv_off += KC * wdt
        for t8 in range(8):
            pb = 2 + (n_ps % 4)
            n_ps += 1
            for kc in range(KC):
                P.op("pe", lambda e, kc=kc, b=b, t8=t8, pb=pb, wdt=wdt: e.matmul(
                    pss[pb][:, 0:wdt], h[:, kc, t8 * 128:(t8 + 1) * 128], wvb[b][:, kc, 0:wdt], start=(kc == 0), stop=(kc == KC - 1)),
                    reads=[f"wvb{b}", hkeys[t8]], writes=[f"ps{pb}"])
            sb_ = nst % 2
            nst += 1
            if nst % 2 == 0:
                P.op("act", lambda e, sb_=sb_, pb=pb, wdt=wdt: e.activation(out=stv[sb_][:, 0:wdt], in_=pss[pb][:, 0:wdt], func=AF.Copy),
                     reads=[f"ps{pb}"], writes=[f"stv{sb_}"])
            else:
                P.op("dve", lambda e, sb_=sb_, pb=pb, wdt=wdt: e.tensor_copy(out=stv[sb_][:, 0:wdt], in_=pss[pb][:, 0:wdt]),
                     reads=[f"ps{pb}"], writes=[f"stv{sb_}"])
            P.dma("sp", outV[t8 * 128:(t8 + 1) * 128, c0:c0 + wdt], stv[sb_][:, 0:wdt], reads=[f"stv{sb_}"], key=f"stvo{sb_}")
    return P.emit()


def k1_host_weights(w_in_l):
    w = w_in_l.reshape(KC, 128, IN_W)
    wfm = np.empty((50, 128, KC * 128), np.float32)
    for ci, c in enumerate(FM_CHUNKS):
        wfm[ci] = w[:, :, c * 128:(c + 1) * 128].transpose(1, 0, 2).reshape(128, KC * 128)
    wvv = w[:, :, V_COLS]
    parts = []
    for (c0, wdt) in V_GROUPS:
        parts.append(wvv[:, :, c0:c0 + wdt].transpose(1, 0, 2).reshape(128, KC * wdt))
    wv = np.concatenate(parts, axis=1)
    return wfm, np.ascontiguousarray(wv)


S = 4096
NEG = -30000.0
EPS = 1e-6
HD = 128


def merge_ops(a, b):
    out, ia, ib = [], 0, 0
    na, nb = len(a), len(b)
    while ia < na or ib < nb:
        if ib >= nb or (ia < na and ia * nb <= ib * na):
            out.append(a[ia]); ia += 1
        else:
            out.append(b[ib]); ib += 1
    return out


def build_k2(parts=("moba", "sb", "swa"), nq_sb=8, nt_tok=32, nheads=None):
    nc = bass.Bass("TRN2", target_bir_lowering=False)
    P = Prog(nc)
    dr = lambda n, sh, dt, kind="ExternalInput": nc.dram_tensor(n, list(sh), dt, kind=kind).ap()
    qk = dr("qk", [13, 128, S], BF16)
    vv = dr("vv", [5, S, HD], BF16)
    mbias = dr("mbias", [2, 2, 128, 512], F32)
    mtab31 = dr("mtab31", [128, 2], F32)
    wbias = dr("wbias", [4, 128, 256], F32)
    sinks = dr("sinks", [128, 4], F32)
    gq = dr("gq", [6, 128, 128], F32)
    gp = dr("gp", [128, 2], F32)
    c_ident = dr("c_ident", [128, 128], BF16)
    c_f32 = dr("c_f32", [3, 128, 128], F32)
    c_sbmask = dr("c_sbmask", [128, 4 * 512], F32)
    oT = dr("oT", [8, 128, S], BF16, kind="ExternalOutput")

    ident = P.sb([128, 128], BF16, "ident")
    cf = P.sb([128, 3, 128], F32, "cf")
    sbmask = P.sb([128, 4, 512], F32, "sbmask")
    qb_ = [P.sb([128, S], BF16, f"qb{i}") for i in range(2)]
    kb_ = [P.sb([128, S], BF16, f"kb{i}") for i in range(2)]
    vb_ = [P.sb([128, 32, HD], BF16, f"vb{i}") for i in range(2)]
    ost = [P.sb([128, S], BF16, f"ost{i}") for i in range(2)]
    mb_sb = P.sb([128, 2, 2, 512], F32, "mb_sb")
    mt31 = P.sb([128, 2], F32, "mt31")
    wb_sb = P.sb([128, 4, 256], F32, "wb_sb")
    sink_sb = P.sb([128, 4], F32, "sink_sb")
    gq_sb = P.sb([128, 6, 128], F32, "gq_sb")
    gp_sb = P.sb([128, 2], F32, "gp_sb")
    pA = [P.ps([128, 512], F32, f"pA{i}") for i in range(2)]
    pZ = [P.ps([128, 512], F32, f"pZ{i}") for i in range(2)]
    pO = P.ps([128, 512], F32, "pO")
    pG = P.ps([128, 512], F32, "pG")
    pTall = P.ps([128, 2, 512], BF16, "pTall")
    pTs = [pTall[:, 0, :], pTall[:, 1, :]]
    P.excl.update(["pTs0", "pTs1"])
    pTh = P.ps([128, 128], BF16, "pTh")

    P.dma("sp", ident[:], c_ident, writes=["ident"])
    P.dma("sp", cf[:], c_f32.rearrange("c p n -> p c n"), writes=["cf"])
    P.dma("sp", sbmask[:], c_sbmask.rearrange("p (o f) -> p o f", o=4), writes=["sbmask"])
    P.dma("sp", mb_sb[:], mbias.rearrange("h q p f -> p h q f"), writes=["mb_sb"])
    P.dma("sp", mt31[:], mtab31, writes=["mt31"])
    P.dma("sp", wb_sb[:], wbias.rearrange("h p f -> p h f"), writes=["wb_sb"])
    P.dma("sp", sink_sb[:], sinks, writes=["sink_sb"])
    P.dma("sp", gq_sb[:], gq.rearrange("h p f -> p h f"), writes=["gq_sb"])
    P.dma("sp", gp_sb[:], gp, writes=["gp_sb"])
    negLge = cf[:, 0, :]
    negones = cf[:, 1, :]
    ones = cf[:, 2, :]

    state = {"nload": 0, "nost": 0}

    def load_qkv(qi, ki, vi):
        b = state["nload"] % 2
        state["nload"] += 1
        if qi is not None:
            P.dma("sp", qb_[b][:], qk[qi], writes=[f"qb{b}"])
        if ki is not None:
            P.dma("sp", kb_[b][:], qk[ki], writes=[f"kb{b}"])
        if vi is not None:
            P.dma("sp", vb_[b][:], vv[vi].rearrange("(t p) d -> p t d", p=128), writes=[f"vb{b}"])
        return b

    sc = {}

    def scr(name, shape, dt):
        if name not in sc:
            sc[name] = P.sb(shape, dt, name)
        return sc[name]

    def head_norm_tok(o_ps, o_key, rden, gidx, ob, q0, tagn):
        on = scr("hn_on", [128, 128], F32)
        junk = scr("hn_junk", [128, 128], F32)
        ss = scr("hn_ss", [128, 1], F32)
        lt = scr("hn_lt", [128, 1], F32)
        rs = scr("hn_rs", [128, 1], F32)
        ob16 = scr("hn_ob16", [128, 128], BF16)
        P.op("dve", lambda e: e.tensor_scalar(out=on[:], in0=o_ps, scalar1=rden, scalar2=None, op0=ALU.mult),
             reads=[o_key, "rden"], writes=["hn_on"])
        P.op("act", lambda e: e.activation(out=junk[:], in_=on[:], func=AF.Square, accum_out=ss[:]),
             reads=["hn_on"], writes=["hn_junk", "hn_ss"])
        P.op("act", lambda e: e.activation(out=lt[:], in_=ss[:], func=AF.Ln, scale=1.0 / HD, bias=EPS),
             reads=["hn_ss"], writes=["hn_lt"])
        P.op("act", lambda e: e.activation(out=rs[:], in_=lt[:], func=AF.Exp, scale=-0.5),
             reads=["hn_lt"], writes=["hn_rs"])
        P.op("dve", lambda e: e.scalar_tensor_tensor(out=ob16[:], in0=on[:], scalar=rs[:], in1=gq_sb[:, gidx, :],
                                                     op0=ALU.mult, op1=ALU.mult),
             reads=["hn_on", "hn_rs", "gq_sb"], writes=["hn_ob16"])
        P.op("pe", lambda e: e.transpose(pTh[:], ob16[:], ident[:]),
             reads=["hn_ob16", "ident"], writes=["pTh"])
        P.op("act", lambda e: e.activation(out=ost[ob][:, q0:q0 + 128], in_=pTh[:], func=AF.Copy),
             reads=["pTh"], writes=[f"ost{ob}"])

    out_idx = 0
    if "moba" in parts:
        for hh in range(2):
            b = load_qkv(2 * hh, 2 * hh + 1, hh)
            q, k, v = qb_[b], kb_[b], vb_[b]
            kq, kk, kv = f"qb{b}", f"kb{b}", f"vb{b}"
            ob = state["nost"] % 2
            state["nost"] += 1
            kms = scr("kms", [128, 16], F32)
            kmean = scr("kmean", [128, 16], BF16)
            P.op("dve", lambda e, k=k: e.tensor_reduce(out=kms[:], in_=k[:].rearrange("p (n j) -> p n j", j=256), axis=AX.X, op=ALU.add),
                 reads=[kk], writes=["kms"])
            P.op("dve", lambda e: e.tensor_scalar(out=kmean[:], in0=kms[:], scalar1=1.0 / 256, scalar2=None, op0=ALU.mult),
                 reads=["kms"], writes=["kmean"])
            srow = scr("srow", [128, S], F32)
            prows = [scr(f"prow{i}", [128, S], BF16) for i in range(2)]
            gate = scr("gate", [128, 16], F32)
            max8 = scr("max8", [128, 8], F32)
            selb = scr("selb", [128, 16], F32)
            cb = scr("cb", [128, 16], F32)
            rmax = scr("rmax", [128, 1], F32)
            rsums = [scr(f"rsum{i}", [128, 1], F32) for i in range(2)]
            rden = scr("rden", [128, 1], F32)
            pts = [scr(f"pts{i}", [128, 512], BF16) for i in range(2)]
            mfronts, mbacks = [], []
            for i in range(nt_tok):
                qbk, qpos = i // 2, i % 2
                q0 = i * 128
                own_w = 128 if qpos == 0 else 256
                L = qbk * 256 + own_w
                qt = q[:, q0:q0 + 128]
                prow, rsum = prows[i % 2], rsums[i % 2]
                kprow, krsum = f"prow{i % 2}", f"rsum{i % 2}"
                _m0 = len(P.ops)
                if qbk >= 4:
                    P.op("pe", lambda e, qt=qt: e.matmul(pG[:, 0:16], qt, kmean[:], start=True, stop=True),
                         reads=[kq, "kmean"], writes=["pG"])
                    P.op("dve", lambda e: e.memset(gate[:], -1e30), writes=["gate"])
                    P.op("dve", lambda e, qbk=qbk: e.tensor_copy(out=gate[:, 0:qbk], in_=pG[:, 0:qbk]),
                         reads=["pG"], writes=["gate"])
                    P.op("dve", lambda e: e.max(out=max8[:], in_=gate[:]), reads=["gate"], writes=["max8"])
                    P.op("dve", lambda e: e.tensor_scalar(out=selb[:], in0=gate[:], scalar1=max8[:, 2:3], scalar2=NEG,
                                                          op0=ALU.is_lt, op1=ALU.mult),
                         reads=["gate", "max8"], writes=["selb"])
                else:
                    P.op("dve", lambda e: e.memset(selb[:], 0.0), writes=["selb"])
                P.op("dve", lambda e, hh=hh: e.tensor_scalar(out=cb[:], in0=selb[:], scalar1=mt31[:, hh:hh + 1], scalar2=None, op0=ALU.add),
                     reads=["selb", "mt31"], writes=["cb"])
                nblk = qbk + 1
                for m in range((nblk + 1) // 2):
                    pb = m % 2
                    n0 = 2 * m
                    wcols = min(512, L - n0 * 256)
                    P.op("pe", lambda e, qt=qt, pb=pb, n0=n0, wcols=wcols, k=k: e.matmul(
                        pA[pb][:, 0:wcols], qt, k[:, n0 * 256:n0 * 256 + wcols], start=True, stop=True),
                        reads=[kq, kk], writes=[f"pA{pb}"])
                    for n in (n0, n0 + 1):
                        if n > qbk:
                            continue
                        c0 = (n - n0) * 256
                        if n < qbk - 1:
                            P.op("dve", lambda e, pb=pb, c0=c0, n=n: e.tensor_scalar(
                                out=srow[:, n * 256:(n + 1) * 256], in0=pA[pb][:, c0:c0 + 256], scalar1=cb[:, n:n + 1], scalar2=None, op0=ALU.add),
                                reads=[f"pA{pb}", "cb"], writes=["srow"])
                        elif n == qbk - 1:
                            P.op("dve", lambda e, pb=pb, c0=c0, n=n, hh=hh, qpos=qpos: e.scalar_tensor_tensor(
                                out=srow[:, n * 256:(n + 1) * 256], in0=pA[pb][:, c0:c0 + 256], scalar=selb[:, n:n + 1],
                                in1=mb_sb[:, hh, qpos, 0:256], op0=ALU.add, op1=ALU.add),
                                reads=[f"pA{pb}", "selb", "mb_sb"], writes=["srow"])
                        else:
                            P.op("dve", lambda e, pb=pb, c0=c0, n=n, hh=hh, qpos=qpos, own_w=own_w: e.tensor_tensor(
                                out=srow[:, n * 256:n * 256 + own_w], in0=pA[pb][:, c0:c0 + own_w],
                                in1=mb_sb[:, hh, qpos, 256:256 + own_w], op=ALU.add),
                                reads=[f"pA{pb}", "mb_sb"], writes=["srow"])
                P.op("dve", lambda e, L=L: e.tensor_reduce(out=rmax[:], in_=srow[:, 0:L], axis=AX.X, op=ALU.max),
                     reads=["srow"], writes=["rmax"])
                P.op("dve", lambda e: e.tensor_scalar(out=rmax[:], in0=rmax[:], scalar1=-1.0, scalar2=None, op0=ALU.mult),
                     reads=["rmax"], writes=["rmax"])
                P.op("act", lambda e, L=L, prow=prow, rsum=rsum: e.activation(out=prow[:, 0:L], in_=srow[:, 0:L], func=AF.Exp, bias=rmax[:], accum_out=rsum[:]),
                     reads=["srow", "rmax"], writes=[kprow, krsum])
                _m1 = len(P.ops)
                P.op("dve", lambda e, rsum=rsum: e.reciprocal(out=rden[:], in_=rsum[:]), reads=[krsum], writes=["rden"])
                nkt = L // 128
                for g0 in range(0, nkt, 4):
                    gb = (g0 // 4) % 2
                    ng = min(4, nkt - g0)
                    for j in range(ng):
                        kt = g0 + j
                        P.op("pe", lambda e, gb=gb, j=j, kt=kt, prow=prow: e.transpose(pTs[gb][:, j * 128:(j + 1) * 128], prow[:, kt * 128:(kt + 1) * 128], ident[:]),
                             reads=[kprow, "ident"], writes=[f"pTs{gb}"])
                    if (g0 // 4) % 2 == 0:
                        P.op("act", lambda e, gb=gb, ng=ng: e.activation(out=pts[gb][:, 0:ng * 128], in_=pTs[gb][:, 0:ng * 128], func=AF.Copy),
                             reads=[f"pTs{gb}"], writes=[f"pts{gb}"])
                    else:
                        P.op("dve", lambda e, gb=gb, ng=ng: e.tensor_copy(out=pts[gb][:, 0:ng * 128], in_=pTs[gb][:, 0:ng * 128]),
                             reads=[f"pTs{gb}"], writes=[f"pts{gb}"])
                    for j in range(ng):
                        kt = g0 + j
                        P.op("pe", lambda e, gb=gb, j=j, kt=kt, nkt=nkt, v=v: e.matmul(
                            pO[:, 0:128], pts[gb][:, j * 128:(j + 1) * 128], v[:, kt, :], start=(kt == 0), stop=(kt == nkt - 1)),
                            reads=[f"pts{gb}", kv], writes=["pO"])
                head_norm_tok(pO[:, 0:128], "pO", rden[:], hh, ob, q0, "m")
                mfronts.append(P.ops[_m0:_m1])
                mbacks.append(P.ops[_m1:])
                del P.ops[_m0:]
            P.ops.extend(mfronts[0])
            for n_ in range(len(mfronts)):
                nxt = mfronts[n_ + 1] if n_ + 1 < len(mfronts) else []
                P.ops.extend(merge_ops(nxt, mbacks[n_]))
            P.dma("sp", oT[out_idx], ost[ob][:], reads=[f"ost{ob}"], key=f"osto{ob}")
            out_idx += 1
    else:
        out_idx = 2

    if "sb" in parts:
        for hh in range(2):
            b = load_qkv(4 + 2 * hh, 5 + 2 * hh, 2 + hh)
            q, k, v = qb_[b], kb_[b], vb_[b]
            kq, kk, kv = f"qb{b}", f"kb{b}", f"vb{b}"
            ob = state["nost"] % 2
            state["nost"] += 1
            ees = [scr(f"sb_e{i}", [128, 512], F32) for i in range(2)]
            azs = [scr(f"sb_az{i}", [128, 512], F32) for i in range(2)]
            spb = [scr(f"sb_sp{i}", [128, 512], F32) for i in range(2)]
            spsum = scr("sb_spsum", [128, 512], F32)
            a32s = [scr(f"sb_a32{i}", [128, 512], F32) for i in range(2)]
            aT = [scr(f"sb_aT{i}", [128, 512], BF16) for i in range(2)]
            osq = scr("sb_osq", [128, 512], F32)
            o32 = scr("sb_o32", [128, 512], F32)
            lnt = scr("sb_lnt", [128, 512], F32)
            rst = scr("sb_rst", [128, 512], F32)
            npair = 0
            for qi in range(nq_sb):
                Q0 = qi * 512
                qt = q[:, Q0:Q0 + 512]
                Jtop = 4 * qi + 3
                fronts, backs = [], []
                for J in range(Jtop, -1, -1):
                    o = J - 4 * qi
                    _mark0 = len(P.ops)
                    pb = npair % 2
                    npair += 1
                    first = (J == Jtop)
                    sp = spb[pb]
                    ksp = f"sb_sp{pb}"
                    ee, az, a32 = ees[pb], azs[pb], a32s[pb]
                    kee, kaz, ka32 = f"sb_e{pb}", f"sb_az{pb}", f"sb_a32{pb}"
                    P.op("pe", lambda e, pb=pb, J=J, qt=qt, k=k: e.matmul(pZ[pb][:], k[:, J * 128:(J + 1) * 128], qt, start=True, stop=True),
                         reads=[kq, kk], writes=[f"pZ{pb}"])
                    P.op("dve", lambda e, pb=pb, az=az: e.tensor_scalar(out=az[:], in0=pZ[pb][:], scalar1=40.0, scalar2=None, op0=ALU.min),
                         reads=[f"pZ{pb}"], writes=[kaz])
                    P.op("act", lambda e, ee=ee, az=az: e.activation(out=ee[:], in_=az[:], func=AF.Exp),
                         reads=[kaz], writes=[kee])
                    P.op("act", lambda e, ee=ee: e.activation(out=ee[:], in_=ee[:], func=AF.Ln, bias=1.0),
                         reads=[kee], writes=[kee])
                    P.op("dve", lambda e, pb=pb, sp=sp, ee=ee: e.tensor_tensor(out=sp[:], in0=pZ[pb][:], in1=ee[:], op=ALU.max),
                         reads=[f"pZ{pb}", kee], writes=[ksp])
                    if o >= 0:
                        P.op("dve", lambda e, sp=sp, o=o: e.tensor_tensor(out=sp[:], in0=sp[:], in1=sbmask[:, o, :], op=ALU.mult),
                             reads=[ksp, "sbmask"], writes=[ksp])
                    _mark1 = len(P.ops)
                    P.op("pe", lambda e, pb=pb, J=J, qt=qt, k=k: e.matmul(pA[pb][:], k[:, J * 128:(J + 1) * 128], qt, start=True, stop=False),
                         reads=[kq, kk], writes=[f"pA{pb}"])
                    if True:
                        P.op("pe", lambda e, pb=pb, sp=sp, first=first: e.matmul(pA[pb][:], negLge, sp[:], start=False, stop=first),
                             reads=[ksp, "cf"], writes=[f"pA{pb}"])
                        if not first:
                            P.op("pe", lambda e, pb=pb: e.matmul(pA[pb][:], negones, spsum[:], start=False, stop=True),
                                 reads=["sb_spsum", "cf"], writes=[f"pA{pb}"])
                    if o >= 0:
                        P.op("act", lambda e, pb=pb, a32=a32: e.activation(out=a32[:], in_=pA[pb][:], func=AF.Exp),
                             reads=[f"pA{pb}"], writes=[ka32])
                        P.op("dve", lambda e, pb=pb, o=o, a32=a32: e.tensor_tensor(out=aT[pb][:], in0=a32[:], in1=sbmask[:, o, :], op=ALU.mult),
                             reads=[ka32, "sbmask"], writes=[f"sb_aT{pb}"])
                    else:
                        P.op("act", lambda e, pb=pb: e.activation(out=aT[pb][:], in_=pA[pb][:], func=AF.Exp),
                             reads=[f"pA{pb}"], writes=[f"sb_aT{pb}"])
                    P.op("pe", lambda e, pb=pb, J=J, Jtop=Jtop, v=v: e.matmul(pO[:], v[:, J, :], aT[pb][:], start=(J == Jtop), stop=(J == 0)),
                         reads=[f"sb_aT{pb}", kv], writes=["pO"])
                    if J > 0:
                        if first:
                            P.op("pool", lambda e, sp=sp: e.tensor_copy(out=spsum[:], in_=sp[:]),
                                 reads=[ksp], writes=["sb_spsum"])
                        else:
                            P.op("pool", lambda e, sp=sp: e.tensor_tensor(out=spsum[:], in0=spsum[:], in1=sp[:], op=ALU.add),
                                 reads=[ksp, "sb_spsum"], writes=["sb_spsum"])
                    fronts.append(P.ops[_mark0:_mark1])
                    backs.append(P.ops[_mark1:])
                    del P.ops[_mark0:]
                P.ops.extend(fronts[0])
                for n_ in range(len(fronts)):
                    nxt = fronts[n_ + 1] if n_ + 1 < len(fronts) else []
                    P.ops.extend(merge_ops(nxt, backs[n_]))
                P.op("dve", lambda e: e.tensor_copy(out=o32[:], in_=pO[:]), reads=["pO"], writes=["sb_o32"])
                P.op("act", lambda e: e.activation(out=osq[:], in_=o32[:], func=AF.Square), reads=["sb_o32"], writes=["sb_osq"])
                P.op("pe", lambda e: e.matmul(pG[:], ones, osq[:], start=True, stop=True), reads=["sb_osq", "cf"], writes=["pG"])
                P.op("act", lambda e: e.activation(out=lnt[:], in_=pG[:], func=AF.Ln, scale=1.0 / HD, bias=EPS), reads=["pG"], writes=["sb_lnt"])
                P.op("act", lambda e: e.activation(out=rst[:], in_=lnt[:], func=AF.Exp, scale=-0.5), reads=["sb_lnt"], writes=["sb_rst"])
                P.op("dve", lambda e, hh=hh, ob=ob, Q0=Q0: e.scalar_tensor_tensor(
                    out=ost[ob][:, Q0:Q0 + 512], in0=o32[:], scalar=gp_sb[:, hh:hh + 1], in1=rst[:], op0=ALU.mult, op1=ALU.mult),
                    reads=["sb_o32", "gp_sb", "sb_rst"], writes=[f"ost{ob}"])
            P.dma("sp", oT[out_idx], ost[ob][:], reads=[f"ost{ob}"], key=f"osto{ob}")
            out_idx += 1
    else:
        out_idx = 4

    if "swa" in parts:
        first_swa = True
        for hh in range(4):
            b = load_qkv(8 + hh, 12 if first_swa else None, 4 if first_swa else None)
            if first_swa:
                kw, vw, kkw, kvw = kb_[b], vb_[b], f"kb{b}", f"vb{b}"
                first_swa = False
            q = qb_[b]
            kq = f"qb{b}"
            ob = state["nost"] % 2
            state["nost"] += 1
            sw = scr("sw_s", [128, 256], F32)
            pws = [scr(f"sw_p{i}", [128, 256], BF16) for i in range(2)]
            nms = [scr(f"sw_nm{i}", [128, 1], F32) for i in range(2)]
            rss = [scr(f"sw_rs{i}", [128, 1], F32) for i in range(2)]
            es = scr("sw_es", [128, 1], F32)
            den = scr("sw_den", [128, 1], F32)
            rden = scr("rden", [128, 1], F32)
            ptw = scr("sw_pt", [128, 256], BF16)
            fronts, backs = [], []
            for i in range(nt_tok):
                q0 = i * 128
                qt = q[:, q0:q0 + 128]
                c_lo = 128 if i == 0 else 0
                k_lo = q0 - 128 + c_lo
                w = 256 - c_lo
                pb = i % 2
                pw, nm, rs = pws[pb], nms[pb], rss[pb]
                kpw, knm, krs = f"sw_p{pb}", f"sw_nm{pb}", f"sw_rs{pb}"
                _m0 = len(P.ops)
                P.op("pe", lambda e, pb=pb, qt=qt, k_lo=k_lo, w=w: e.matmul(pA[pb][:, 0:w], qt, kw[:, k_lo:k_lo + w], start=True, stop=True),
                     reads=[kq, kkw], writes=[f"pA{pb}"])
                P.op("dve", lambda e, pb=pb, hh=hh, c_lo=c_lo, w=w: e.tensor_tensor(out=sw[:, 0:w], in0=pA[pb][:, 0:w], in1=wb_sb[:, hh, c_lo:256], op=ALU.add),
                     reads=[f"pA{pb}", "wb_sb"], writes=["sw_s"])
                P.op("dve", lambda e, w=w, nm=nm: e.tensor_reduce(out=nm[:], in_=sw[:, 0:w], axis=AX.X, op=ALU.max),
                     reads=["sw_s"], writes=[knm])
                P.op("dve", lambda e, hh=hh, nm=nm: e.tensor_scalar(out=nm[:], in0=nm[:], scalar1=sink_sb[:, hh:hh + 1], scalar2=-1.0, op0=ALU.max, op1=ALU.mult),
                     reads=[knm, "sink_sb"], writes=[knm])
                P.op("act", lambda e, w=w, pw=pw, nm=nm, rs=rs: e.activation(out=pw[:, 0:w], in_=sw[:, 0:w], func=AF.Exp, bias=nm[:], accum_out=rs[:]),
                     reads=["sw_s", knm], writes=[kpw, krs])
                _m1 = len(P.ops)
                P.op("act", lambda e, hh=hh, nm=nm: e.activation(out=es[:], in_=sink_sb[:, hh:hh + 1], func=AF.Exp, bias=nm[:]),
                     reads=["sink_sb", knm], writes=["sw_es"])
                P.op("dve", lambda e, rs=rs: e.tensor_tensor(out=den[:], in0=rs[:], in1=es[:], op=ALU.add),
                     reads=[krs, "sw_es"], writes=["sw_den"])
                P.op("dve", lambda e: e.reciprocal(out=rden[:], in_=den[:]), reads=["sw_den"], writes=["rden"])
                nkt = w // 128
                for j in range(nkt):
                    P.op("pe", lambda e, j=j, pw=pw: e.transpose(pTs[0][:, j * 128:(j + 1) * 128], pw[:, j * 128:(j + 1) * 128], ident[:]),
                         reads=[kpw, "ident"], writes=["pTs0"])
                P.op("act", lambda e, w=w: e.activation(out=ptw[:, 0:w], in_=pTs[0][:, 0:w], func=AF.Copy),
                     reads=["pTs0"], writes=["sw_pt"])
                for j in range(nkt):
                    kt = (k_lo // 128) + j
                    P.op("pe", lambda e, j=j, kt=kt, nkt=nkt: e.matmul(pO[:, 0:128], ptw[:, j * 128:(j + 1) * 128], vw[:, kt, :], start=(j == 0), stop=(j == nkt - 1)),
                         reads=["sw_pt", kvw], writes=["pO"])
                head_norm_tok(pO[:, 0:128], "pO", rden[:], 2 + hh, ob, q0, "w")
                fronts.append(P.ops[_m0:_m1])
                backs.append(P.ops[_m1:])
                del P.ops[_m0:]
            P.ops.extend(fronts[0])
            for n_ in range(len(fronts)):
                nxt = fronts[n_ + 1] if n_ + 1 < len(fronts) else []
                P.ops.extend(merge_ops(nxt, backs[n_]))
            P.dma("sp", oT[out_idx], ost[ob][:], reads=[f"ost{ob}"], key=f"osto{ob}")
            out_idx += 1
    return P.emit()


D = 4096
KC = 32
TOK = 1024
TP = 512
EPS = 1e-6
NVEC = 7


def build_k3(NH, moe=False, npass=2, ngroups=None):
    NG = NH // 2
    if ngroups is None:
        ngroups = NG
    nc = bass.Bass("TRN2", target_bir_lowering=False)
    P = Prog(nc)
    dr = lambda n, sh, dt, kind="ExternalInput": nc.dram_tensor(n, list(sh), dt, kind=kind).ap()
    oT = dr("oT", [KC, 128, TOK], BF16)
    xT = dr("xT", [D, TOK], F32)
    vecs = dr("vecs", [128, NVEC * KC], F32)
    wout = dr("wout", [KC, 128, KC * 128], F32)
    wg = dr("wg", [NH, 128, KC * 128], F32)
    wu = dr("wu", [NH, 128, KC * 128], F32)
    wd = dr("wd", [NG, 2, 128, D], F32)
    if moe:
        wr = dr("wr", [128, KC * 8], F32)
        c_sel = dr("c_sel", [8, 8 * 128], F32)
        c_identf = dr("c_identf", [128, 128], F32)
    x1T = dr("x1T", [D, TOK], F32, kind="ExternalOutput")
    x2T = dr("x2T", [D, TOK], F32, kind="ExternalOutput")

    ones = P.sb([128, 128], F32, "ones")
    vec = P.sb([128, NVEC * KC], F32, "vec")
    gA = P.sb([128, KC], F32, "gA")
    gB = P.sb([128, KC], F32, "gB")
    gC = P.sb([128, KC], F32, "gC")
    hbuf = P.sb([128, KC, TP], BF16, "hbuf")
    yacc = P.sb([128, KC, TP], F32, "yacc")
    wgb = [P.sb([128, KC, 128], BF16, f"wgb{i}") for i in range(2)]
    wub = [P.sb([128, KC, 128], BF16, f"wub{i}") for i in range(2)]
    wdb = [P.sb([128, 2, D], BF16, f"wdb{i}") for i in range(2)]
    xc = [P.sb([128, TP], F32, f"xc{i}") for i in range(2)]
    sq = [P.sb([128, TP], F32, f"sq{i}") for i in range(2)]
    lnt = P.sb([128, TP], F32, "lnt")
    rstd = P.sb([128, TP], F32, "rstd")
    t1 = [P.sb([128, TP], F32, f"t1_{i}") for i in range(2)]
    sg = [P.sb([128, TP], F32, f"sg{i}") for i in range(2)]
    act = [P.sb([128, 2, TP], BF16, f"act{i}") for i in range(2)]
    pss = [P.ps([128, 512], F32, f"ps{i}") for i in range(8)]
    if moe:
        wr_sb = P.sb([128, KC, 8], F32, "wr_sb")
        sel = P.sb([8, 8 * 128], F32, "sel")
        identf = P.sb([128, 128], F32, "identf")
        lg = P.sb([128, 8], F32, "lg")
        mx8 = P.sb([128, 8], F32, "mx8")
        dd = P.sb([128, 1], F32, "dd")
        w1 = P.sb([128, 1], F32, "w1")
        w2 = P.sb([128, 1], F32, "w2")
        m1 = P.sb([128, 8], F32, "m1")
        comb = P.sb([128, 8], F32, "comb")
        combT = P.sb([8, TP], F32, "combT")
        combe = P.sb([128, TP], F32, "combe")
        P.dma("sp", wr_sb[:], wr.rearrange("p (kc e) -> p kc e", e=8), writes=["wr_sb"])
        P.dma("sp", sel[:], c_sel, writes=["sel"])
        P.dma("sp", identf[:], c_identf, writes=["identf"])

    P.op("pool", lambda e: e.memset(ones[:], 1.0), writes=["ones"])
    P.dma("sp", vec[:], vecs, writes=["vec"])
    V = lambda i: vec[:, i * KC:(i + 1) * KC]
    P.op("dve", lambda e: e.tensor_tensor(out=gA[:], in0=V(0), in1=V(1), op=ALU.mult), reads=["vec"], writes=["gA"])
    P.op("dve", lambda e: e.tensor_scalar(out=gB[:], in0=V(3), scalar1=1.0, scalar2=None, op0=ALU.add), reads=["vec"], writes=["gB"])
    P.op("dve", lambda e: e.tensor_tensor(out=gB[:], in0=gB[:], in1=V(2), op=ALU.mult), reads=["vec", "gB"], writes=["gB"])
    P.op("dve", lambda e: e.tensor_tensor(out=gC[:], in0=V(5), in1=V(6), op=ALU.mult), reads=["vec"], writes=["gC"])

    xT_v = xT.rearrange("(kc p) t -> p kc t", p=128)
    x1T_v = x1T.rearrange("(kc p) t -> p kc t", p=128)
    x2T_v = x2T.rearrange("(kc p) t -> p kc t", p=128)
    cnt = {"ps": 0, "w": 0, "ev": 0}

    def next_ps():
        cnt["ps"] += 1
        return 2 + (cnt["ps"] % 6)

    def rms_rstd(src_fn, key_fn):
        for kc in range(KC):
            b = kc % 2
            P.op("act", lambda e, kc=kc, b=b: e.activation(out=sq[b][:], in_=src_fn(kc), func=AF.Square),
                 reads=[key_fn(kc)], writes=[f"sq{b}"])
            P.op("pe", lambda e, kc=kc, b=b: e.matmul(pss[0][:], ones[:], sq[b][:], start=(kc == 0), stop=(kc == KC - 1)),
                 reads=["ones", f"sq{b}"], writes=["ps0"])
        P.op("act", lambda e: e.activation(out=lnt[:], in_=pss[0][:], func=AF.Ln, scale=1.0 / D, bias=EPS), reads=["ps0"], writes=["lnt"])
        P.op("act", lambda e: e.activation(out=rstd[:], in_=lnt[:], func=AF.Exp, scale=-0.5), reads=["lnt"], writes=["rstd"])

    for ps_ in range(npass):
        T0 = ps_ * TP
        P.dma("sp", hbuf[:], oT.rearrange("kc p t -> p kc t")[:, :, T0:T0 + TP], writes=["hbuf"])
        for c in range(KC):
            b = cnt["w"] % 2
            cnt["w"] += 1
            P.dma("pool", wgb[b][:], wout[c].rearrange("p (kc n) -> p kc n", n=128), writes=[f"wgb{b}"])
            pb = next_ps()
            for kc in range(KC):
                P.op("pe", lambda e, kc=kc, b=b, pb=pb: e.matmul(pss[pb][:], wgb[b][:, kc, :], hbuf[:, kc, :], start=(kc == 0), stop=(kc == KC - 1)),
                     reads=[f"wgb{b}", "hbuf"], writes=[f"ps{pb}"])
            if c % 2 == 0:
                P.op("act", lambda e, c=c, pb=pb: e.activation(out=yacc[:, c, :], in_=pss[pb][:], func=AF.Copy), reads=[f"ps{pb}"], writes=[f"y{c}"])
            else:
                P.op("dve", lambda e, c=c, pb=pb: e.tensor_copy(out=yacc[:, c, :], in_=pss[pb][:]), reads=[f"ps{pb}"], writes=[f"y{c}"])
        rms_rstd(lambda kc: yacc[:, kc, :], lambda kc: f"y{kc}")
        for kc in range(KC):
            b = kc % 2
            P.dma("sp", xc[b][:], xT_v[:, kc, T0:T0 + TP], writes=[f"xc{b}"])
            P.op("dve", lambda e, kc=kc, b=b: e.tensor_tensor(out=t1[b][:], in0=yacc[:, kc, :], in1=rstd[:], op=ALU.mult),
                 reads=[f"y{kc}", "rstd"], writes=[f"t1_{b}"])
            P.op("dve", lambda e, kc=kc, b=b: e.scalar_tensor_tensor(out=yacc[:, kc, :], in0=t1[b][:], scalar=gA[:, kc:kc + 1], in1=xc[b][:],
                                                                    op0=ALU.mult, op1=ALU.add),
                 reads=[f"t1_{b}", "gA", f"xc{b}"], writes=[f"y{kc}"])
            P.dma("sp", x1T_v[:, kc, T0:T0 + TP], yacc[:, kc, :], reads=[f"y{kc}"], writes=["x1dram"], key="x1dram")
        rms_rstd(lambda kc: yacc[:, kc, :], lambda kc: f"y{kc}")
        if moe:
            pass
        for kc in range(KC):
            b = kc % 2
            P.op("dve", lambda e, kc=kc, b=b: e.scalar_tensor_tensor(out=t1[b][:], in0=yacc[:, kc, :], scalar=gB[:, kc:kc + 1], in1=rstd[:],
                                                                    op0=ALU.mult, op1=ALU.mult),
                 reads=[f"y{kc}", "gB", "rstd"], writes=[f"t1_{b}"])
            if moe:
                P.op("act", lambda e, kc=kc, b=b: e.activation(out=sq[b][:], in_=t1[b][:], func=AF.Identity, bias=vec[:, 4 * KC + kc:4 * KC + kc + 1]),
                     reads=[f"t1_{b}", "vec"], writes=[f"sq{b}"])
                P.op("pool", lambda e, kc=kc, b=b: e.tensor_copy(out=hbuf[:, kc, :], in_=sq[b][:]), reads=[f"sq{b}"], writes=["hbuf"])
                for tt in range(4):
                    P.op("pe", lambda e, kc=kc, b=b, tt=tt: e.matmul(pss[1 + tt][:, 0:8], sq[b][:, tt * 128:(tt + 1) * 128], wr_sb[:, kc, :],
                                                                     start=(kc == 0), stop=(kc == KC - 1)),
                         reads=[f"sq{b}", "wr_sb"], writes=[f"ps{1 + tt}"])
            else:
                P.op("act", lambda e, kc=kc, b=b: e.activation(out=hbuf[:, kc, :], in_=t1[b][:], func=AF.Identity, bias=vec[:, 4 * KC + kc:4 * KC + kc + 1]),
                     reads=[f"t1_{b}", "vec"], writes=["hbuf"])
        if moe:
            for tt in range(4):
                P.op("dve", lambda e, tt=tt: e.tensor_copy(out=lg[:], in_=pss[1 + tt][:, 0:8]), reads=[f"ps{1 + tt}"], writes=["lg"])
                P.op("dve", lambda e: e.max(out=mx8[:], in_=lg[:]), reads=["lg"], writes=["mx8"])
                P.op("dve", lambda e: e.tensor_tensor(out=dd[:], in0=mx8[:, 1:2], in1=mx8[:, 0:1], op=ALU.subtract), reads=["mx8"], writes=["dd"])
                P.op("act", lambda e: e.activation(out=w2[:], in_=dd[:], func=AF.Exp), reads=["dd"], writes=["w2"])
                P.op("dve", lambda e: e.tensor_scalar(out=w1[:], in0=w2[:], scalar1=1.0, scalar2=None, op0=ALU.add), reads=["w2"], writes=["w1"])
                P.op("dve", lambda e: e.reciprocal(out=w1[:], in_=w1[:]), reads=["w1"], writes=["w1"])
                P.op("dve", lambda e: e.tensor_tensor(out=w2[:], in0=w2[:], in1=w1[:], op=ALU.mult), reads=["w1", "w2"], writes=["w2"])
                P.op("dve", lambda e: e.tensor_scalar(out=m1[:], in0=lg[:], scalar1=mx8[:, 0:1], scalar2=w1[:], op0=ALU.is_equal, op1=ALU.mult),
                     reads=["lg", "mx8", "w1"], writes=["m1"])
                P.op("dve", lambda e: e.tensor_scalar(out=comb[:], in0=lg[:], scalar1=mx8[:, 1:2], scalar2=w2[:], op0=ALU.is_equal, op1=ALU.mult),
                     reads=["lg", "mx8", "w2"], writes=["comb"])
                P.op("dve", lambda e: e.tensor_tensor(out=comb[:], in0=comb[:], in1=m1[:], op=ALU.add), reads=["comb", "m1"], writes=["comb"])
                P.op("pe", lambda e: e.transpose(pss[0][0:8, 0:128], comb[:], identf[:]), reads=["comb", "identf"], writes=["ps0"])
                P.op("act", lambda e, tt=tt: e.activation(out=combT[:, tt * 128:(tt + 1) * 128], in_=pss[0][0:8, 0:128], func=AF.Copy),
                     reads=["ps0"], writes=["combT"])
        for gi in range(ngroups):
            b = gi % 2
            P.dma("pool", wdb[b][:], wd[gi].rearrange("j p n -> p j n"), writes=[f"wdb{b}"])
            if moe and gi % 16 == 0:
                ex = gi // 16
                P.op("pe", lambda e, ex=ex: e.matmul(pss[1][:], sel[:, ex * 128:(ex + 1) * 128], combT[:], start=True, stop=True),
                     reads=["sel", "combT"], writes=["ps1"])
                P.op("act", lambda e: e.activation(out=combe[:], in_=pss[1][:], func=AF.Copy), reads=["ps1"], writes=["combe"])
            ab = gi % 2
            for jj in range(2):
                wb_ = cnt["w"] % 2
                cnt["w"] += 1
                P.dma("pool", wgb[wb_][:], wg[2 * gi + jj].rearrange("p (kc n) -> p kc n", n=128), writes=[f"wgb{wb_}"])
                P.dma("pool", wub[wb_][:], wu[2 * gi + jj].rearrange("p (kc n) -> p kc n", n=128), writes=[f"wub{wb_}"])
                pg = next_ps()
                for kc in range(KC):
                    P.op("pe", lambda e, kc=kc, wb_=wb_, pg=pg: e.matmul(pss[pg][:], wgb[wb_][:, kc, :], hbuf[:, kc, :],
                                                                         start=(kc == 0), stop=(kc == KC - 1)),
                         reads=[f"wgb{wb_}", "hbuf"], writes=[f"ps{pg}"])
                pu = next_ps()
                for kc in range(KC):
                    P.op("pe", lambda e, kc=kc, wb_=wb_, pu=pu: e.matmul(pss[pu][:], wub[wb_][:, kc, :], hbuf[:, kc, :],
                                                                         start=(kc == 0), stop=(kc == KC - 1)),
                         reads=[f"wub{wb_}", "hbuf"], writes=[f"ps{pu}"])
                P.op("act", lambda e, jj=jj, pg=pg: e.activation(out=sg[jj][:], in_=pss[pg][:], func=AF.Silu), reads=[f"ps{pg}"], writes=[f"sg{jj}"])
                if moe:
                    P.op("dve", lambda e, jj=jj, pu=pu: e.tensor_tensor(out=sg[jj][:], in0=sg[jj][:], in1=pss[pu][:], op=ALU.mult),
                         reads=[f"sg{jj}", f"ps{pu}"], writes=[f"sg{jj}"])
                    P.op("dve", lambda e, jj=jj, ab=ab: e.tensor_tensor(out=act[ab][:, jj, :], in0=sg[jj][:], in1=combe[:], op=ALU.mult),
                         reads=[f"sg{jj}", "combe"], writes=[f"act{ab}"])
                else:
                    P.op("dve", lambda e, jj=jj, pu=pu, ab=ab: e.tensor_tensor(out=act[ab][:, jj, :], in0=sg[jj][:], in1=pss[pu][:], op=ALU.mult),
                         reads=[f"sg{jj}", f"ps{pu}"], writes=[f"act{ab}"])
            for c in range(KC):
                pd = next_ps()
                for jj in range(2):
                    P.op("pe", lambda e, c=c, jj=jj, pd=pd, b=b, ab=ab: e.matmul(pss[pd][:], wdb[b][:, jj, c * 128:(c + 1) * 128], act[ab][:, jj, :],
                                                                                 start=(jj == 0), stop=(jj == 1)),
                         reads=[f"wdb{b}", f"act{ab}"], writes=[f"ps{pd}"])
                if gi == 0:
                    P.op("dve", lambda e, c=c, pd=pd: e.tensor_copy(out=yacc[:, c, :], in_=pss[pd][:]), reads=[f"ps{pd}"], writes=[f"y{c}"])
                else:
                    P.op("dve", lambda e, c=c, pd=pd: e.tensor_tensor(out=yacc[:, c, :], in0=yacc[:, c, :], in1=pss[pd][:], op=ALU.add),
                         reads=[f"ps{pd}", f"y{c}"], writes=[f"y{c}"])
        rms_rstd(lambda kc: yacc[:, kc, :], lambda kc: f"y{kc}")
        for kc in range(KC):
            b = kc % 2
            P.dma("sp", xc[b][:], x1T_v[:, kc, T0:T0 + TP], reads=["x1dram"], writes=[f"xc{b}"])
            P.op("dve", lambda e, kc=kc, b=b: e.tensor_tensor(out=t1[b][:], in0=yacc[:, kc, :], in1=rstd[:], op=ALU.mult),
                 reads=[f"y{kc}", "rstd"], writes=[f"t1_{b}"])
            P.op("dve", lambda e, kc=kc, b=b: e.scalar_tensor_tensor(out=t1[b][:], in0=t1[b][:], scalar=gC[:, kc:kc + 1], in1=xc[b][:],
                                                                    op0=ALU.mult, op1=ALU.add),
                 reads=[f"t1_{b}", "gC", f"xc{b}"], writes=[f"t1_{b}"])
            P.dma("sp", x2T_v[:, kc, T0:T0 + TP], t1[b][:], reads=[f"t1_{b}"], key=f"xoo{b}")
    return P.emit()


def tile_cols(w, width):
    K, N = w.shape
    return np.ascontiguousarray(w.reshape(K // 128, 128, N // width, width).transpose(2, 1, 0, 3).reshape(N // width, 128, (K // 128) * width))


from concourse.bass_utils import run_bass_kernel_spmd

N_CORES = 8
_PROGS = {}


def _prog(name, fn):
    if name not in _PROGS:
        _PROGS[name] = fn()
    return _PROGS[name]


def _lay(v):
    return np.asarray(v, np.float32).reshape(KC, 128).T


def _run(nc, in_maps):
    res = run_bass_kernel_spmd(nc, in_maps, core_ids=list(range(N_CORES)))
    return res.results


def kernel(x, c, rel_bias, w_ada, b_ada, g_pre_mix, w_in, g_grp_moba, g_grp_sb, g_grp_swa, swa_sinks, w_out,
           g_post_mix, g_pre_ffn, w_ff_gate, w_ff_up, w_ff_down, w_router, w_moe_gate, w_moe_up, w_moe_down, g_post_ffn):
    f32 = np.float32
    x = np.asarray(x, f32)
    c = np.asarray(c, f32)
    rel_bias = np.asarray(rel_bias, f32)
    w_ada = np.asarray(w_ada, f32)
    b_ada = np.asarray(b_ada, f32)
    B, S_, D_ = x.shape
    oh = build_onehot()
    cT = np.ascontiguousarray(c.T.reshape(KC, 128, 2).transpose(1, 0, 2).reshape(128, KC * 2))
    rbaug = np.concatenate([rel_bias, np.full((1, 24), NEG, f32)], axis=0)
    ims = []
    for core in range(N_CORES):
        sl = slice(core * ADA_CORE, (core + 1) * ADA_CORE)
        wt = w_ada[:, :, sl].reshape(2, KC, 128, 6, 512).transpose(0, 3, 2, 1, 4).reshape(12, 128, KC * 512)
        ims.append({"cT": cT, "wada": np.ascontiguousarray(wt),
                    "bada": np.ascontiguousarray(np.broadcast_to(b_ada[:, sl].reshape(1, 2 * ADA_CORE), (2, 2 * ADA_CORE))),
                    "rbaug": rbaug, "oh": np.ascontiguousarray(oh[:, core * N_TAB_CORE:(core + 1) * N_TAB_CORE])})
    r0 = _run(_prog("k0", build_k0), ims)
    del ims
    mod = np.concatenate([np.asarray(r["modo"]) for r in r0], axis=2)
    tab = np.concatenate([np.asarray(r["tabo"]) for r in r0], axis=1)
    wtab = tab[:, 0:128 * 256].reshape(24, 128, 256)
    mtab = tab[:, 128 * 256:].reshape(24, 2, 128, 512)
    consts2 = {}
    consts2["c_ident"] = np.eye(128, dtype=f32).astype(ml_dtypes.bfloat16)
    jp = np.arange(128)[:, None]
    jj = np.arange(128)[None, :]
    consts2["c_f32"] = np.stack([-(jp >= jj).astype(f32), -np.ones((128, 128), f32), np.ones((128, 128), f32)])
    ff = np.arange(512)[None, :]
    consts2["c_sbmask"] = np.ascontiguousarray(np.concatenate([(ff > 128 * o + jp).astype(f32) for o in range(4)], axis=1))
    selc = np.zeros((8, 8 * 128), f32)
    for e_ in range(8):
        selc[e_, e_ * 128:(e_ + 1) * 128] = 1

    xcur = x
    for l in range(2):
        sh_m, sc_m, gt_m, sh_f, sc_f, gt_f = [mod[l][:, i * D:(i + 1) * D] for i in range(6)]
        wfm, wv = k1_host_weights(np.asarray(w_in[l], f32))
        ims = []
        for core in range(N_CORES):
            b, ts = core // 4, core % 4
            xT = np.ascontiguousarray(xcur[b, ts * TOK:(ts + 1) * TOK, :].T)
            vecs = np.ascontiguousarray(np.concatenate([_lay(g_pre_mix[l]), _lay(sc_m[b]), _lay(sh_m[b])], axis=1))
            ims.append({"xT": xT, "vecs": vecs, "wfm": wfm, "wv": wv})
        r1 = _run(_prog("k1", build_k1), ims)
        del ims, wfm, wv
        QK = [np.concatenate([np.asarray(r1[b * 4 + ts]["outT"]) for ts in range(4)], axis=2) for b in range(B)]
        VV = [np.concatenate([np.asarray(r1[b * 4 + ts]["outV"]) for ts in range(4)], axis=0) for b in range(B)]
        del r1
        ims = []
        for core in range(N_CORES):
            b, g = core // 4, core % 4
            qi = []
            for hh in range(2):
                qi += [2 * g + hh, 8 + 2 * g + hh]
            for hh in range(2):
                qi += [16 + 2 * g + hh, 24 + 2 * g + hh]
            qi += [32 + 4 * g + i for i in range(4)]
            qi += [48 + g // 2]
            qk = np.ascontiguousarray(QK[b][qi])
            vcols = [(2 * g) * 128, (2 * g + 1) * 128, 1024 + (2 * g) * 128, 1024 + (2 * g + 1) * 128, 2048 + (g // 2) * 128]
            vv = np.ascontiguousarray(np.stack([VV[b][:, c0:c0 + 128] for c0 in vcols]))
            mh = [2 * g, 2 * g + 1]
            wh = [4 * g + i for i in range(4)]
            gq = np.stack([np.broadcast_to(np.asarray(g_grp_moba[l], f32)[h * 128:(h + 1) * 128][None], (128, 128)) for h in mh] +
                          [np.broadcast_to(np.asarray(g_grp_swa[l], f32)[h * 128:(h + 1) * 128][None], (128, 128)) for h in wh])
            gp = np.ascontiguousarray(np.stack([np.asarray(g_grp_sb[l], f32)[h * 128:(h + 1) * 128] for h in mh], axis=1))
            im = {"qk": qk, "vv": vv,
                  "mbias": np.ascontiguousarray(mtab[mh]),
                  "mtab31": np.ascontiguousarray(np.broadcast_to(rel_bias[31, mh][None, :], (128, 2))),
                  "wbias": np.ascontiguousarray(wtab[[8 + h for h in wh]]),
                  "sinks": np.ascontiguousarray(np.broadcast_to(np.asarray(swa_sinks[l], f32)[wh][None, :], (128, 4))),
                  "gq": np.ascontiguousarray(gq.astype(f32)), "gp": gp,
                  "c_ident": consts2["c_ident"], "c_f32": consts2["c_f32"], "c_sbmask": consts2["c_sbmask"]}
            ims.append(im)
        r2 = _run(_prog("k2", build_k2), ims)
        del ims, QK, VV
        OT = []
        for b in range(B):
            ch = []
            for h in range(8):
                ch.append(np.asarray(r2[b * 4 + h // 2]["oT"])[h % 2])
            for h in range(8):
                ch.append(np.asarray(r2[b * 4 + h // 2]["oT"])[2 + h % 2])
            for h in range(16):
                ch.append(np.asarray(r2[b * 4 + h // 4]["oT"])[4 + h % 4])
            OT.append(np.stack(ch))
        del r2
        wout_t = tile_cols(np.asarray(w_out[l], f32), 128)
        i = l // 2
        if l % 2 == 0:
            wg_t = tile_cols(np.asarray(w_ff_gate[i], f32), 128)
            wu_t = tile_cols(np.asarray(w_ff_up[i], f32), 128)
            wd_t = np.ascontiguousarray(np.asarray(w_ff_down[i], f32).reshape(-1, 2, 128, D))
            prog = _prog("k3d", lambda: build_k3(112, moe=False))
            extra = {}
        else:
            wg_t = np.concatenate([tile_cols(np.asarray(w_moe_gate[i, e_], f32), 128) for e_ in range(8)], axis=0)
            wu_t = np.concatenate([tile_cols(np.asarray(w_moe_up[i, e_], f32), 128) for e_ in range(8)], axis=0)
            wd_t = np.ascontiguousarray(np.asarray(w_moe_down[i], f32).reshape(-1, 2, 128, D))
            prog = _prog("k3m", lambda: build_k3(256, moe=True))
            extra = {"wr": np.ascontiguousarray(np.asarray(w_router[i], f32).reshape(KC, 128, 8).transpose(1, 0, 2).reshape(128, KC * 8)),
                     "c_sel": selc, "c_identf": np.eye(128, dtype=f32)}
        ims = []
        for core in range(N_CORES):
            b, ts = core // 4, core % 4
            xT = np.ascontiguousarray(xcur[b, ts * TOK:(ts + 1) * TOK, :].T)
            vecs = np.ascontiguousarray(np.concatenate(
                [_lay(g_post_mix[l]), _lay(gt_m[b]), _lay(g_pre_ffn[l]), _lay(sc_f[b]), _lay(sh_f[b]), _lay(g_post_ffn[l]), _lay(gt_f[b])], axis=1))
            im = {"oT": np.ascontiguousarray(OT[b][:, :, ts * TOK:(ts + 1) * TOK]), "xT": xT, "vecs": vecs,
                  "wout": wout_t, "wg": wg_t, "wu": wu_t, "wd": wd_t}
            im.update(extra)
            ims.append(im)
        r3 = _run(prog, ims)
        del ims, wg_t, wu_t, wd_t, wout_t, OT
        xn = np.empty((B, S_, D_), f32)
        for core in range(N_CORES):
            b, ts = core // 4, core % 4
            xn[b, ts * TOK:(ts + 1) * TOK, :] = np.asarray(r3[core]["x2T"]).T
        del r3
        xcur = xn
    return xcur
```
